# Optimizing a Trainium2 kernel written in Bass

```python
import math
import jax, jax.numpy as jnp
from jax import lax
import numpy as np

D_MODEL = 1024
BATCH = 1
SEQ = 16384
DEPTH = 4

F32 = jnp.float32
N_MIXERS = 3
ROPE_THETA = 10000.0
LN_EPS = 1e-5
RMS_EPS = 1e-6
Q_BLOCK = 128
NEG = -1e30
ALPHA_DN = (2 * DEPTH) ** 0.25
BETA_DN = (8 * DEPTH) ** -0.25

DA_HEADS = 8
DA_HEAD_DIM = D_MODEL // (2 * DA_HEADS)
MLA_HEADS = 16
MLA_NOPE = 64
MLA_ROPE = 32
MLA_V = 64
MLA_Q_RANK = 256
MLA_KV_RANK = 128
NSA_HEADS = 16
NSA_GROUPS = 4
NSA_HEAD_DIM = 64
NSA_CMP_LEN = 32
NSA_CMP_STRIDE = 16
NSA_CMP_HIDDEN = 256
NSA_SLC_LEN = 64
NSA_SLC_TOPK = 16
NSA_WINDOW = 512
NSA_FORCE = 1e9
N_EXPERTS = 32
TOP_K = 4
D_EXPERT = 1024
SWIGLU_LIMIT = 7.0
SWIGLU_ALPHA = 1.702
MOE_BLOCK = 128

kernel_name = 'hybrid_diff_mla_nsa_moe_deepnorm'


def layer_norm(x, g, b):
    xf = x.astype(F32)
    mu = jnp.mean(xf, -1, keepdims=True)
    var = jnp.mean(jnp.square(xf - mu), -1, keepdims=True)
    return ((xf - mu) * lax.rsqrt(var + LN_EPS) * g.astype(F32) + b.astype(F32)).astype(x.dtype)


def rms_norm(x, g, eps):
    xf = x.astype(F32)
    return (xf * lax.rsqrt(jnp.mean(xf * xf, -1, keepdims=True) + eps) * g.astype(F32)).astype(x.dtype)


def rope_angles(pos, dim):
    inv = ROPE_THETA ** (-jnp.arange(0, dim, 2, dtype=F32) / dim)
    ang = pos.astype(F32)[:, None] * inv[None, :]
    return jnp.cos(ang), jnp.sin(ang)


def apply_rope(t, cos, sin):
    c = cos[None, :, None, :].astype(t.dtype)
    s = sin[None, :, None, :].astype(t.dtype)
    t1, t2 = jnp.split(t, 2, axis=-1)
    return jnp.concatenate([t1 * c - t2 * s, t2 * c + t1 * s], axis=-1)


def to_blocks(t):
    B, S = t.shape[:2]
    return jnp.moveaxis(t.reshape(B, S // Q_BLOCK, Q_BLOCK, *t.shape[2:]), 1, 0)


def from_blocks(t):
    nb, B, qb = t.shape[:3]
    return jnp.moveaxis(t, 0, 1).reshape(B, nb * qb, *t.shape[3:])


def causal_mask(bi, n_keys):
    qpos = bi * Q_BLOCK + jnp.arange(Q_BLOCK)
    return jnp.arange(n_keys)[None, :] <= qpos[:, None]


def diff_attention(x, w_in, lq1, lk1, lq2, lk2, subln_g, w_out, cos, sin, layer_idx):
    B, S, _ = x.shape
    H, d = DA_HEADS, DA_HEAD_DIM
    q, k, v = jnp.split(x @ w_in, 3, axis=-1)
    q = apply_rope(q.reshape(B, S, 2 * H, d), cos, sin)
    k = apply_rope(k.reshape(B, S, 2 * H, d), cos, sin)
    v = v.reshape(B, S, H, 2 * d)
    lam_init = 0.8 - 0.6 * math.exp(-0.3 * layer_idx)
    lam = (jnp.exp(jnp.sum(lq1.astype(F32) * lk1.astype(F32)))
           - jnp.exp(jnp.sum(lq2.astype(F32) * lk2.astype(F32))) + lam_init)
    scale = d ** -0.5

    def block(args):
        qi, bi = args
        s = jnp.einsum('bqhd,bkhd->bhqk', qi, k, preferred_element_type=F32) * scale
        s = jnp.where(causal_mask(bi, S), s, NEG)
        p = jax.nn.softmax(s, axis=-1).reshape(B, H, 2, Q_BLOCK, S)
        p = p[:, :, 0] - lam * p[:, :, 1]
        return jnp.einsum('bhqk,bkhe->bqhe', p.astype(v.dtype), v)

    o = from_blocks(lax.map(block, (to_blocks(q), jnp.arange(S // Q_BLOCK))))
    o = rms_norm(o, subln_g, LN_EPS) * (1.0 - lam_init)
    return o.reshape(B, S, H * 2 * d) @ w_out


def mla(x, w_in, q_norm_g, kv_norm_g, w_uq, w_ukv, w_out, cos, sin):
    B, S, _ = x.shape
    H = MLA_HEADS
    c_q, c_kv, k_rope = jnp.split(x @ w_in, [MLA_Q_RANK, MLA_Q_RANK + MLA_KV_RANK], axis=-1)
    q = (rms_norm(c_q, q_norm_g, RMS_EPS) @ w_uq).reshape(B, S, H, MLA_NOPE + MLA_ROPE)
    q_nope, q_rope = jnp.split(q, [MLA_NOPE], axis=-1)
    q_rope = apply_rope(q_rope, cos, sin)
    k_rope = apply_rope(k_rope[:, :, None, :], cos, sin)[:, :, 0]
    kv = (rms_norm(c_kv, kv_norm_g, RMS_EPS) @ w_ukv).reshape(B, S, H, MLA_NOPE + MLA_V)
    k_nope, v = jnp.split(kv, [MLA_NOPE], axis=-1)
    scale = (MLA_NOPE + MLA_ROPE) ** -0.5

    def block(args):
        qn, qr, bi = args
        s = (jnp.einsum('bqhd,bkhd->bhqk', qn, k_nope, preferred_element_type=F32)
             + jnp.einsum('bqhr,bkr->bhqk', qr, k_rope, preferred_element_type=F32)) * scale
        s = jnp.where(causal_mask(bi, S), s, NEG)
        p = jax.nn.softmax(s, axis=-1)
        return jnp.einsum('bhqk,bkhd->bqhd', p.astype(v.dtype), v)

    o = from_blocks(lax.map(block, (to_blocks(q_nope), to_blocks(q_rope), jnp.arange(S // Q_BLOCK))))
    return o.reshape(B, S, H * MLA_V) @ w_out


def nsa(x, w_in, pos_k, pos_v, ck_w1, ck_w2, cv_w1, cv_w2, w_out, cos, sin):
    B, S, _ = x.shape
    H, G, d = NSA_HEADS, NSA_GROUPS, NSA_HEAD_DIM
    R = H // G
    L, st, Ls, W = NSA_CMP_LEN, NSA_CMP_STRIDE, NSA_SLC_LEN, NSA_WINDOW
    n_cmp = (S - L) // st + 1
    n_slc = S // Ls
    n_top = min(NSA_SLC_TOPK, n_slc)
    ratio = Ls // st
    off = L // st - 1
    sizes = [H * d] + [G * d] * 6 + [3 * H]
    q, kc, vc, ks, vs, kw, vw, gl = jnp.split(x @ w_in, np.cumsum(sizes)[:-1].tolist(), axis=-1)
    q = apply_rope(q.reshape(B, S, H, d), cos, sin).reshape(B, S, G, R, d)
    ks = apply_rope(ks.reshape(B, S, G, d), cos, sin)
    kw = apply_rope(kw.reshape(B, S, G, d), cos, sin)
    vs = vs.reshape(B, S, G, d)
    vw = vw.reshape(B, S, G, d)
    gate = jax.nn.sigmoid(gl.astype(F32)).reshape(B, S, G, R, 3)

    blk_idx = jnp.arange(n_cmp)[:, None] * st + jnp.arange(L)[None, :]

    def compress(t, pos, w1, w2):
        tb = t.reshape(B, S, G, d)[:, blk_idx] + pos[:, None, :]
        tb = tb.transpose(0, 1, 3, 2, 4).reshape(B, n_cmp, G, L * d)
        return jax.nn.gelu(tb @ w1) @ w2

    cmp_end = jnp.arange(n_cmp) * st + L - 1
    cos_c, sin_c = rope_angles(cmp_end, d)
    k_cmp = apply_rope(compress(kc, pos_k, ck_w1, ck_w2), cos_c, sin_c)
    v_cmp = compress(vc, pos_v, cv_w1, cv_w2)

    ksT = ks.transpose(0, 2, 1, 3)
    vsT = vs.transpose(0, 2, 1, 3)
    kw_pad = jnp.pad(kw, ((0, 0), (W, 0), (0, 0), (0, 0)))
    vw_pad = jnp.pad(vw, ((0, 0), (W, 0), (0, 0), (0, 0)))
    b_ix = jnp.arange(B)[:, None, None, None]
    g_ix = jnp.arange(G)[None, :, None, None]
    scale = d ** -0.5
    back = ratio * n_slc - n_cmp
    jb = jnp.arange(n_slc)

    def block(args):
        qi, gi, bi = args
        qpos = bi * Q_BLOCK + jnp.arange(Q_BLOCK)
        s = jnp.einsum('bqgrd,bngd->bgrqn', qi, k_cmp, preferred_element_type=F32) * scale
        m_c = cmp_end[None, :] <= qpos[:, None]
        p_c = jax.nn.softmax(jnp.where(m_c, s, NEG), axis=-1) * jnp.any(m_c, -1).astype(F32)[:, None]
        o_c = jnp.einsum('bgrqn,bngd->bqgrd', p_c.astype(v_cmp.dtype), v_cmp)
        p_g = jnp.pad(p_c.sum(axis=2), ((0, 0), (0, 0), (0, 0), (off, back)))
        imp = jnp.zeros(p_g.shape[:3] + (n_slc,), F32)
        for mm in range(ratio):
            for nn in range(L // st):
                start = off + mm - nn
                imp = imp + lax.slice_in_dim(p_g, start, start + ratio * (n_slc - 1) + 1, stride=ratio, axis=-1)
        cur = qpos // Ls
        valid = jb[None, :] <= cur[:, None]
        forced = valid & ((jb[None, :] == 0) | (jb[None, :] >= cur[:, None] - 1))
        score = jnp.where(forced, NSA_FORCE, jnp.where(valid, imp, -1.0))
        _, sel = lax.top_k(score, n_top)
        tok = (sel[..., None] * Ls + jnp.arange(Ls)).reshape(B, G, Q_BLOCK, n_top * Ls)
        k_sel = ksT[b_ix, g_ix, tok]
        v_sel = vsT[b_ix, g_ix, tok]
        s = jnp.einsum('bqgrd,bgqtd->bgrqt', qi, k_sel, preferred_element_type=F32) * scale
        m_s = (tok <= qpos[None, None, :, None])[:, :, None]
        p_s = jax.nn.softmax(jnp.where(m_s, s, NEG), axis=-1)
        o_s = jnp.einsum('bgrqt,bgqtd->bqgrd', p_s.astype(v_sel.dtype), v_sel)
        k_w = lax.dynamic_slice_in_dim(kw_pad, bi * Q_BLOCK, W + Q_BLOCK, axis=1)
        v_w = lax.dynamic_slice_in_dim(vw_pad, bi * Q_BLOCK, W + Q_BLOCK, axis=1)
        kpos = bi * Q_BLOCK - W + jnp.arange(W + Q_BLOCK)
        m_w = ((kpos[None, :] <= qpos[:, None]) & (kpos[None, :] > qpos[:, None] - W)
               & (kpos[None, :] >= 0))
        s = jnp.einsum('bqgrd,bkgd->bgrqk', qi, k_w, preferred_element_type=F32) * scale
        p_w = jax.nn.softmax(jnp.where(m_w, s, NEG), axis=-1)
        o_w = jnp.einsum('bgrqk,bkgd->bqgrd', p_w.astype(v_w.dtype), v_w)
        o = gi[..., 0:1] * o_c + gi[..., 1:2] * o_s + gi[..., 2:3] * o_w
        return o.astype(qi.dtype)

    o = from_blocks(lax.map(block, (to_blocks(q), to_blocks(gate), jnp.arange(S // Q_BLOCK))))
    return o.reshape(B, S, H * d) @ w_out


def moe(x, w_router, b_router, w_gu, b_gu, w_down, b_down):
    B, S, Dm = x.shape
    N = B * S
    xt = x.reshape(N, Dm)
    logits = (xt @ w_router + b_router).astype(F32)
    top_logit, top_e = lax.top_k(logits, TOP_K)
    gate = jax.nn.softmax(top_logit, axis=-1)
    A = N * TOP_K
    e_flat = top_e.reshape(A)
    tok_flat = jnp.arange(A, dtype=jnp.int32) // TOP_K
    g_flat = gate.reshape(A)
    order = jnp.argsort(e_flat)
    e_sorted = e_flat[order]
    counts = jnp.zeros((N_EXPERTS,), jnp.int32).at[e_flat].add(1)
    starts = jnp.cumsum(counts) - counts
    padded = (counts + MOE_BLOCK - 1) // MOE_BLOCK * MOE_BLOCK
    pends = jnp.cumsum(padded)
    pstarts = pends - padded
    dest = pstarts[e_sorted] + (jnp.arange(A, dtype=jnp.int32) - starts[e_sorted])
    n_rows = (A + MOE_BLOCK - 1) // MOE_BLOCK * MOE_BLOCK + N_EXPERTS * MOE_BLOCK
    row_tok = jnp.full((n_rows,), N, jnp.int32).at[dest].set(tok_flat[order])
    row_gate = jnp.zeros((n_rows,), F32).at[dest].set(g_flat[order])
    nblk = n_rows // MOE_BLOCK
    blk_e = jnp.minimum(jnp.searchsorted(pends, jnp.arange(nblk, dtype=jnp.int32) * MOE_BLOCK, side='right'),
                        N_EXPERTS - 1)
    x_pad = jnp.concatenate([xt, jnp.zeros((1, Dm), xt.dtype)], axis=0)

    def expert_block(args):
        tok, e, gw = args
        hgu = x_pad[tok] @ w_gu[e] + b_gu[e]
        hg = jnp.minimum(hgu[:, 0::2], SWIGLU_LIMIT)
        hl = jnp.clip(hgu[:, 1::2], -SWIGLU_LIMIT, SWIGLU_LIMIT)
        act = hg * jax.nn.sigmoid(SWIGLU_ALPHA * hg) * (hl + 1.0)
        y = act @ w_down[e] + b_down[e]
        return y * gw[:, None].astype(y.dtype)

    y = lax.map(expert_block, (row_tok.reshape(nblk, MOE_BLOCK), blk_e, row_gate.reshape(nblk, MOE_BLOCK)))
    out = jnp.zeros((N + 1, Dm), y.dtype).at[row_tok].add(y.reshape(n_rows, Dm))
    return out[:N].reshape(B, S, Dm).astype(x.dtype)


def setup_inputs(seed: int = 0) -> dict:
    key = jax.random.key(seed)
    k = jax.random.split(key, 32)

    def nrm(i, shape, scale):
        return scale * jax.random.normal(k[i], shape, F32)

    nA = len(range(0, DEPTH, N_MIXERS))
    nB = len(range(1, DEPTH, N_MIXERS))
    nC = len(range(2, DEPTH, N_MIXERS))
    D = D_MODEL
    da_v = DA_HEADS * 2 * DA_HEAD_DIM
    mla_in = MLA_Q_RANK + MLA_KV_RANK + MLA_ROPE
    nsa_in = NSA_HEADS * NSA_HEAD_DIM + 6 * NSA_GROUPS * NSA_HEAD_DIM + 3 * NSA_HEADS
    cmp_in = NSA_CMP_LEN * NSA_HEAD_DIM
    return {
        'x': nrm(0, (BATCH, SEQ, D), 1.0),
        'da_w_in': nrm(1, (nA, D, 3 * da_v), D ** -0.5),
        'da_lambda_q1': nrm(2, (nA, DA_HEAD_DIM), 0.1),
        'da_lambda_k1': nrm(3, (nA, DA_HEAD_DIM), 0.1),
        'da_lambda_q2': nrm(4, (nA, DA_HEAD_DIM), 0.1),
        'da_lambda_k2': nrm(5, (nA, DA_HEAD_DIM), 0.1),
        'da_subln': 1.0 + nrm(6, (nA, 2 * DA_HEAD_DIM), 0.02),
        'da_w_out': nrm(7, (nA, da_v, D), BETA_DN * da_v ** -0.5),
        'mla_w_in': nrm(8, (nB, D, mla_in), D ** -0.5),
        'mla_q_norm': 1.0 + nrm(9, (nB, MLA_Q_RANK), 0.02),
        'mla_kv_norm': 1.0 + nrm(10, (nB, MLA_KV_RANK), 0.02),
        'mla_w_uq': nrm(11, (nB, MLA_Q_RANK, MLA_HEADS * (MLA_NOPE + MLA_ROPE)), MLA_Q_RANK ** -0.5),
        'mla_w_ukv': nrm(12, (nB, MLA_KV_RANK, MLA_HEADS * (MLA_NOPE + MLA_V)), MLA_KV_RANK ** -0.5),
        'mla_w_out': nrm(13, (nB, MLA_HEADS * MLA_V, D), BETA_DN * (MLA_HEADS * MLA_V) ** -0.5),
        'nsa_w_in': nrm(14, (nC, D, nsa_in), D ** -0.5),
        'nsa_cmp_pos_k': nrm(15, (nC, NSA_CMP_LEN, NSA_HEAD_DIM), 0.02),
        'nsa_cmp_pos_v': nrm(16, (nC, NSA_CMP_LEN, NSA_HEAD_DIM), 0.02),
        'nsa_cmp_k_w1': nrm(17, (nC, cmp_in, NSA_CMP_HIDDEN), cmp_in ** -0.5),
        'nsa_cmp_k_w2': nrm(18, (nC, NSA_CMP_HIDDEN, NSA_HEAD_DIM), NSA_CMP_HIDDEN ** -0.5),
        'nsa_cmp_v_w1': nrm(19, (nC, cmp_in, NSA_CMP_HIDDEN), cmp_in ** -0.5),
        'nsa_cmp_v_w2': nrm(20, (nC, NSA_CMP_HIDDEN, NSA_HEAD_DIM), NSA_CMP_HIDDEN ** -0.5),
        'nsa_w_out': nrm(21, (nC, NSA_HEADS * NSA_HEAD_DIM, D), BETA_DN * (NSA_HEADS * NSA_HEAD_DIM) ** -0.5),
        'ln1_g': 1.0 + nrm(22, (DEPTH, D), 0.02),
        'ln1_b': nrm(23, (DEPTH, D), 0.02),
        'ln2_g': 1.0 + nrm(24, (DEPTH, D), 0.02),
        'ln2_b': nrm(25, (DEPTH, D), 0.02),
        'moe_w_router': nrm(26, (DEPTH, D, N_EXPERTS), D ** -0.5),
        'moe_b_router': nrm(27, (DEPTH, N_EXPERTS), 0.01),
        'moe_w_gu': nrm(28, (DEPTH, N_EXPERTS, D, 2 * D_EXPERT), D ** -0.5),
        'moe_b_gu': nrm(29, (DEPTH, N_EXPERTS, 2 * D_EXPERT), 0.02),
        'moe_w_down': nrm(30, (DEPTH, N_EXPERTS, D_EXPERT, D), BETA_DN * D_EXPERT ** -0.5),
        'moe_b_down': nrm(31, (DEPTH, N_EXPERTS, D), 0.02),
    }


def reference(x, da_w_in, da_lambda_q1, da_lambda_k1, da_lambda_q2, da_lambda_k2, da_subln, da_w_out,
              mla_w_in, mla_q_norm, mla_kv_norm, mla_w_uq, mla_w_ukv, mla_w_out,
              nsa_w_in, nsa_cmp_pos_k, nsa_cmp_pos_v, nsa_cmp_k_w1, nsa_cmp_k_w2, nsa_cmp_v_w1,
              nsa_cmp_v_w2, nsa_w_out,
              ln1_g, ln1_b, ln2_g, ln2_b,
              moe_w_router, moe_b_router, moe_w_gu, moe_b_gu, moe_w_down, moe_b_down):
    S = x.shape[1]
    pos = jnp.arange(S)
    cos64, sin64 = rope_angles(pos, DA_HEAD_DIM)
    cos32, sin32 = rope_angles(pos, MLA_ROPE)
    h = x
    for i in range(DEPTH):
        m, j = i % N_MIXERS, i // N_MIXERS
        if m == 0:
            y = diff_attention(h, da_w_in[j], da_lambda_q1[j], da_lambda_k1[j], da_lambda_q2[j],
                               da_lambda_k2[j], da_subln[j], da_w_out[j], cos64, sin64, i)
        elif m == 1:
            y = mla(h, mla_w_in[j], mla_q_norm[j], mla_kv_norm[j], mla_w_uq[j], mla_w_ukv[j],
                    mla_w_out[j], cos32, sin32)
        else:
            y = nsa(h, nsa_w_in[j], nsa_cmp_pos_k[j], nsa_cmp_pos_v[j], nsa_cmp_k_w1[j],
                    nsa_cmp_k_w2[j], nsa_cmp_v_w1[j], nsa_cmp_v_w2[j], nsa_w_out[j], cos64, sin64)
        h = layer_norm(ALPHA_DN * h + y, ln1_g[i], ln1_b[i])
        y = moe(h, moe_w_router[i], moe_b_router[i], moe_w_gu[i], moe_b_gu[i], moe_w_down[i], moe_b_down[i])
        h = layer_norm(ALPHA_DN * h + y, ln2_g[i], ln2_b[i])
    return h
```

```python
import contextlib
import numpy as np
import concourse.bass as bass
import concourse.mybir as mybir

F32 = mybir.dt.float32
BF16 = mybir.dt.bfloat16
I32 = mybir.dt.int32
AF = mybir.ActivationFunctionType
ALU = mybir.AluOpType
AX = mybir.AxisListType


class Buf:
    __slots__ = ("t", "name", "lw", "rd", "excl")

    def __init__(self, t, name, excl=False):
        self.t = t
        self.name = name
        self.excl = excl
        self.lw = None
        self.rd = []

    def __getitem__(self, idx):
        return self.t[idx]


class KB:
    ENG = ("pe", "act", "dve", "pool", "sp")

    def __init__(self, n_dma_sems=12):
        self.nc = bass.Bass("TRN2", target_bir_lowering=False)
        self.es = contextlib.ExitStack()
        nc = self.nc
        self.eng = {"pe": nc.tensor, "act": nc.scalar, "dve": nc.vector,
                    "pool": nc.gpsimd, "sp": nc.sync}
        self.sems = {}
        self.cnt = {}
        for e in self.ENG:
            self.sems[e] = self.es.enter_context(nc.semaphore("s_" + e))
            self.cnt[e] = 0
        self.waited = {e: {} for e in self.ENG}
        self.dsem = {}
        self.dsem_i = {}
        for q in ("sp", "pool", "act"):
            lst = []
            for i in range(n_dma_sems):
                key = "d_%s_%d" % (q, i)
                self.sems[key] = self.es.enter_context(nc.semaphore(key))
                self.cnt[key] = 0
                lst.append(key)
            self.dsem[q] = lst
            self.dsem_i[q] = 0
        self.nbuf = 0
        self.ninstr = 0

    def sb(self, shape, dtype, name=None):
        self.nbuf += 1
        name = ("sb%d_" % self.nbuf) + (name or "")
        t = self.es.enter_context(self.nc.sbuf_tensor(name, list(shape), dtype))
        return Buf(t, name)

    def ps(self, shape, dtype=F32, name=None):
        self.nbuf += 1
        name = ("ps%d_" % self.nbuf) + (name or "")
        t = self.es.enter_context(self.nc.psum_tensor(name, list(shape), dtype))
        return Buf(t, name, excl=True)

    def dram(self, name, shape, dtype, kind):
        t = self.nc.dram_tensor(name, list(shape), dtype, kind=kind)
        return Buf(t.ap(), name)

    def _wait(self, e, tok):
        if tok is None:
            return
        key, val = tok
        w = self.waited[e]
        if w.get(key, 0) >= val:
            return
        self.eng[e].wait_ge(self.sems[key], val)
        w[key] = val

    def _deps(self, e, R, W, same_engine_sync):
        toks = []
        for b in R:
            if b.lw is not None:
                toks.append(b.lw)
        for b in W:
            if b.lw is not None:
                toks.append(b.lw)
            toks.extend(b.rd)
        for tok in toks:
            if tok[0] == e and not same_engine_sync:
                continue
            self._wait(e, tok)

    def op(self, e, fn, R=(), W=(), sync_same=None):
        if sync_same is None:
            sync_same = (e != "pe")
        if e != "pe":
            xr = [b for b in R if b.excl and b not in W]
            if xr:
                W = list(W) + xr
        self._deps(e, R, W, sync_same)
        ins = fn(self.eng[e])
        self.cnt[e] += 1
        self.ninstr += 1
        tok = (e, self.cnt[e])
        ins.then_inc(self.sems[e], 1)
        for b in W:
            b.lw = tok
            b.rd = []
        for b in R:
            if b not in W:
                b.rd.append(tok)
                if len(b.rd) > 64:
                    b.rd = self._compact(b.rd)
        return tok

    @staticmethod
    def _compact(rd):
        best = {}
        for k, v in rd:
            if best.get(k, 0) < v:
                best[k] = v
        return list(best.items())

    def dma(self, q, out_ap, in_ap, R=(), W=(), **kw):
        self._deps(q, R, W, True)
        lst = self.dsem[q]
        key = lst[self.dsem_i[q] % len(lst)]
        self.dsem_i[q] += 1
        if self.cnt[key] > 0:
            self._wait(q, (key, self.cnt[key]))
        ins = self.eng[q].dma_start(out=out_ap, in_=in_ap, **kw)
        self.cnt[key] += 16
        self.ninstr += 1
        tok = (key, self.cnt[key])
        ins.then_inc(self.sems[key], 16)
        for b in W:
            b.lw = tok
            b.rd = []
        for b in R:
            if b not in W:
                b.rd.append(tok)
        return tok

    def fillreg(self, val):
        c = getattr(self, "_fregs", None)
        if c is None:
            c = self._fregs = {}
        if val not in c:
            c[val] = self.nc.gpsimd.to_reg(val)
        return c[val]

    def barrier(self):
        toks = [(e, self.cnt[e]) for e in self.ENG if self.cnt[e] > 0]
        for q in self.dsem:
            for key in self.dsem[q]:
                if self.cnt[key] > 0:
                    toks.append((key, self.cnt[key]))
        for e in self.ENG:
            for tok in toks:
                if tok[0] != e:
                    self._wait(e, tok)

    def push_scope(self):
        self._outer = getattr(self, "_outer", [])
        self._outer.append(self.es)
        self.es = contextlib.ExitStack()

    def pop_scope(self):
        self.barrier()
        self.es.close()
        self.es = self._outer.pop()

    def finish(self, bufs):
        for b in bufs:
            if b.lw is not None:
                self._wait("sp", b.lw)

    def close(self):
        self.es.close()


ALPHA = 8.0 ** 0.25
LN_EPS = 1e-5
D = 1024
NE = 32
FE = 1024


def make_ident(k, dtype, name):
    idt = k.sb([128, 128], dtype, name)
    k.op("pool", lambda e: e.memset(idt[:], 1.0), W=[idt])
    k.op("pool", lambda e: e.affine_select(out=idt[:], in_=idt[:], pattern=[[-1, 128]],
                                           compare_op=ALU.is_equal, fill=k.fillreg(0.0), base=0,
                                           channel_multiplier=1), R=[idt], W=[idt])
    return idt


def bcast_load(k, q, dst, src_ap_1d, n):
    pass


def layer_norm_block(k, Zb, zap, g_t, b_t, tmp):
    st, mv, rstd = tmp
    for c in range(2):
        k.op("dve", lambda e, c=c: e.bn_stats(out=st[:, c, :], in_=zap[:, c * 512:(c + 1) * 512]),
             R=[Zb], W=[st])
    k.op("dve", lambda e: e.bn_aggr(out=mv[:], in_=st[:].rearrange("p a b -> p (a b)")), R=[st], W=[mv])
    k.op("act", lambda e: e.activation(out=rstd[:], in_=mv[:, 1:2], func=AF.Sqrt, bias=eps_ap(k), scale=1.0),
         R=[mv], W=[rstd])
    k.op("dve", lambda e: e.reciprocal(out=rstd[:], in_=rstd[:]), R=[rstd], W=[rstd])
    k.op("dve", lambda e: e.tensor_scalar(out=zap, in0=zap, scalar1=mv[:, 0:1], scalar2=rstd[:, 0:1],
                                          op0=ALU.subtract, op1=ALU.mult), R=[Zb, mv, rstd], W=[Zb])
    k.op("pool", lambda e: e.tensor_tensor(out=zap, in0=zap, in1=g_t[:], op=ALU.mult), R=[Zb, g_t], W=[Zb])
    k.op("pool", lambda e: e.tensor_tensor(out=zap, in0=zap, in1=b_t[:], op=ALU.add), R=[Zb, b_t], W=[Zb])


_EPS = {}


def eps_ap(k):
    if getattr(k, "_eps_t", None) is None:
        t = k.sb([128, 1], F32, "eps_c")
        k.op("pool", lambda e: e.memset(t[:], LN_EPS), W=[t])
        k._eps_t = t
    return k._eps_t[:]


def emit_post(k, NT, OT, h_d, wout_d, ln1g_d, ln1b_d, ln2g_d, ln2b_d, wr_d, br_d,
              wgu_d, bgu_d, wd_d, bd_d, out_d, ne=NE, psum=None):
    T = NT * 128
    XT = OT
    OTb = [Buf(XT.t, "otb%d" % i) for i in range(NT)]
    if XT.lw is not None:
        for b in OTb:
            b.lw = XT.lw
    ident_f = make_ident(k, F32, "ident_f")
    lng = k.sb([128, D], F32, "lng")
    lnb = k.sb([128, D], F32, "lnb")
    k.dma("sp", lng[:], ln1g_d.t.partition_broadcast(128), R=[ln1g_d], W=[lng])
    k.dma("sp", lnb[:], ln1b_d.t.partition_broadcast(128), R=[ln1b_d], W=[lnb])
    wr = k.sb([128, 8, NE], F32, "wr")
    k.dma("sp", wr[:], wr_d.t.rearrange("(c p) e -> p c e", p=128), R=[wr_d], W=[wr])
    brb = k.sb([128, NE], F32, "brb")
    k.dma("sp", brb[:], br_d.t.partition_broadcast(128), R=[br_d], W=[brb])
    bd = k.sb([NE, D], F32, "bd")
    k.dma("sp", bd[:], bd_d.t, R=[bd_d], W=[bd])
    bguT = k.sb([128, 8, 2, NE], F32, "bguT")
    if psum is None:
        psum = [k.ps([128, 512], F32, "pb%d" % i) for i in range(8)]
    pg, pl, py, pm = psum[0:2], psum[2:4], psum[4:6], psum[6:8]
    import os
    STAGE = int(os.environ.get("STAGE", "9"))
    SUB = int(os.environ.get("SUB", "9"))
    NW = 10
    wgu_ring = [k.sb([128, 2 * FE], BF16, "wgu%d" % i) for i in range(NW)]
    wd_ring = [k.sb([128, D], BF16, "wd%d" % i) for i in range(NW)]
    ring_i = {"gu": 0, "d": 0}

    def load_gu(src_ap_fn):
        bufs = []
        for kc in range(8):
            b = wgu_ring[ring_i["gu"] % NW]
            ring_i["gu"] += 1
            bufs.append(b)
        return bufs

    wo = []
    for kc in range(8):
        b = wd_ring[ring_i["d"] % NW]
        ring_i["d"] += 1
        k.dma("pool", b[:], wout_d.t[kc * 128:(kc + 1) * 128, :], R=[wout_d], W=[b])
        wo.append(b)

    Z = [k.sb([128, D], F32, "Z%d" % i) for i in range(NT)]
    G = k.sb([128, NT, NE], F32, "G")
    st = k.sb([128, 2, 6], F32, "ln_st")
    mv = k.sb([128, 2], F32, "ln_mv")
    rstd = k.sb([128, 1], F32, "ln_rstd")
    lntmp = (st, mv, rstd)
    eps_ap(k)
    k.push_scope()
    bgu_raw = k.sb([NE, 2 * FE], F32, "bgu_raw")
    k.dma("sp", bgu_raw[:], bgu_d.t, R=[bgu_d], W=[bgu_raw])
    for c in range(8):
        for gl in range(2):
            pb = pm[(c * 2 + gl) % 2]
            k.op("pe", lambda e, c=c, gl=gl, pb=pb: e.transpose(
                out=pb[:, 0:NE], in_=bgu_raw[:, 2 * 128 * c + gl: 2 * 128 * (c + 1): 2],
                identity=ident_f[0:NE, 0:NE]), R=[bgu_raw, ident_f], W=[pb])
            k.op("dve", lambda e, c=c, gl=gl, pb=pb: e.tensor_copy(out=bguT[:, c, gl, :], in_=pb[:, 0:NE]),
                 R=[pb], W=[bguT])

    xT32 = [k.sb([128, 8, 128], F32, "xT32_%d" % i) for i in range(2)]
    L = k.sb([128, NE], F32, "L")
    M8 = k.sb([128, 8], F32, "M8")
    nm = k.sb([128, 1], F32, "nm")
    msk = k.sb([128, NE], F32, "msk")
    Ee = k.sb([128, NE], F32, "Ee")
    ssum = k.sb([128, 1], F32, "ssum")
    GT = [k.sb([NE, 128], F32, "GT%d" % i) for i in range(2)]

    for tb in range(NT if STAGE >= 2 else 0):
        Zb = Z[tb]
        k.dma("sp", Zb[:], h_d.t[tb * 128:(tb + 1) * 128, :], R=[h_d], W=[Zb])
        for half in range(2):
            pb = py[half]
            for kc in range(8):
                k.op("pe", lambda e, kc=kc, half=half, pb=pb: e.matmul(
                    pb[:], lhsT=XT.t[:, kc, tb * 128:(tb + 1) * 128], rhs=wo[kc][:, half * 512:(half + 1) * 512],
                    start=(kc == 0), stop=(kc == 7)), R=[OTb[tb], wo[kc]], W=[pb])
            k.op("dve", lambda e, half=half, pb=pb: e.scalar_tensor_tensor(
                out=Zb[:, half * 512:(half + 1) * 512], in0=Zb[:, half * 512:(half + 1) * 512],
                scalar=ALPHA, in1=pb[:], op0=ALU.mult, op1=ALU.add), R=[Zb, pb], W=[Zb])
        if SUB < 2:
            continue
        layer_norm_block(k, Zb, Zb[:], lng, lnb, lntmp)
        if SUB < 3:
            continue
        x32 = xT32[tb % 2]
        for c4 in range(2):
            pb = pm[c4]
            for j in range(4):
                c = c4 * 4 + j
                k.op("pe", lambda e, c=c, j=j, pb=pb: e.transpose(
                    out=pb[:, j * 128:(j + 1) * 128], in_=Zb[:, c * 128:(c + 1) * 128],
                    identity=ident_f[:]), R=[Zb, ident_f], W=[pb])
            k.op("act", lambda e, c4=c4, pb=pb: e.activation(
                out=XT.t[:, c4 * 4:(c4 + 1) * 4, tb * 128:(tb + 1) * 128],
                in_=pb[:].rearrange("p (j t) -> p j t", j=4),
                func=AF.Copy), R=[pb], W=[OTb[tb]])
            k.op("dve", lambda e, c4=c4, pb=pb: e.tensor_copy(
                out=x32[:, c4 * 4:(c4 + 1) * 4, :], in_=pb[:].rearrange("p (j t) -> p j t", j=4)),
                R=[pb], W=[x32])
        if SUB < 4:
            continue
        pb = pm[0]
        for c in range(8):
            k.op("pe", lambda e, c=c, pb=pb: e.matmul(pb[:, 0:NE], lhsT=x32[:, c, :], rhs=wr[:, c, :],
                                                      start=(c == 0), stop=(c == 7)),
                 R=[x32, wr], W=[pb])
        k.op("dve", lambda e, pb=pb: e.tensor_tensor(out=L[:], in0=pb[:, 0:NE], in1=brb[:], op=ALU.add),
             R=[pb, brb], W=[L])
        k.op("dve", lambda e: e.max(out=M8[:], in_=L[:]), R=[L], W=[M8])
        k.op("dve", lambda e: e.tensor_scalar(out=msk[:], in0=L[:], scalar1=M8[:, 3:4], scalar2=None,
                                              op0=ALU.is_ge), R=[L, M8], W=[msk])
        k.op("dve", lambda e: e.tensor_scalar(out=nm[:], in0=M8[:, 0:1], scalar1=-1.0, scalar2=None,
                                              op0=ALU.mult), R=[M8], W=[nm])
        k.op("act", lambda e: e.activation(out=Ee[:], in_=L[:], func=AF.Exp, bias=nm[:], scale=1.0),
             R=[L, nm], W=[Ee])
        k.op("dve", lambda e: e.tensor_tensor(out=Ee[:], in0=Ee[:], in1=msk[:], op=ALU.mult),
             R=[Ee, msk], W=[Ee])
        k.op("dve", lambda e: e.reduce_sum(out=ssum[:], in_=Ee[:], axis=AX.X), R=[Ee], W=[ssum])
        k.op("dve", lambda e: e.reciprocal(out=ssum[:], in_=ssum[:]), R=[ssum], W=[ssum])
        k.op("dve", lambda e: e.tensor_scalar(out=G[:, tb, :], in0=Ee[:], scalar1=ssum[:, 0:1], scalar2=None,
                                              op0=ALU.mult), R=[Ee, ssum], W=[G])
        if SUB < 5:
            continue
        gt = GT[tb % 2]
        pb = pm[1]
        k.op("pe", lambda e, pb=pb: e.transpose(out=pb[0:NE, 0:128], in_=G[:, tb, :], identity=ident_f[:]),
             R=[G, ident_f], W=[pb])
        k.op("act", lambda e, pb=pb: e.activation(out=gt[:], in_=pb[0:NE, 0:128], func=AF.Copy),
             R=[pb], W=[gt])
        for half in range(2):
            pb = py[half]
            k.op("pe", lambda e, half=half, pb=pb: e.matmul(
                pb[:], lhsT=gt[:], rhs=bd[:, half * 512:(half + 1) * 512], start=True, stop=True),
                R=[gt, bd], W=[pb])
            k.op("dve", lambda e, half=half, pb=pb: e.scalar_tensor_tensor(
                out=Zb[:, half * 512:(half + 1) * 512], in0=Zb[:, half * 512:(half + 1) * 512],
                scalar=ALPHA, in1=pb[:], op0=ALU.mult, op1=ALU.add), R=[Zb, pb], W=[Zb])

    k.pop_scope()
    XT.lw = OTb[NT - 1].lw
    XT.rd = []
    k.dma("sp", lng[:], ln2g_d.t.partition_broadcast(128), R=[ln2g_d], W=[lng])
    k.dma("sp", lnb[:], ln2b_d.t.partition_broadcast(128), R=[ln2b_d], W=[lnb])

    TT = 512 if T >= 512 else T
    NTT = T // TT
    NSUB = TT // 128
    hg = [k.sb([128, TT], F32, "hg%d" % i) for i in range(2)]
    sg = [k.sb([128, TT], F32, "sg%d" % i) for i in range(2)]
    hl = [k.sb([128, TT], F32, "hl%d" % i) for i in range(2)]
    actT = [k.sb([128, 8, TT], BF16, "actT%d" % i) for i in range(2)]
    ui = 0
    ai = 0
    for ex in range(ne if STAGE >= 4 else 0):
        gu = []
        for kc in range(8):
            b = wgu_ring[ring_i["gu"] % NW]
            ring_i["gu"] += 1
            k.dma("pool", b[:], wgu_d.t[ex, kc * 128:(kc + 1) * 128, :], R=[wgu_d], W=[b])
            gu.append(b)
        dn = []
        for kc in range(8):
            b = wd_ring[ring_i["d"] % NW]
            ring_i["d"] += 1
            k.dma("pool", b[:], wd_d.t[ex, kc * 128:(kc + 1) * 128, :], R=[wd_d], W=[b])
            dn.append(b)
        for tt in range(NTT):
            at = actT[ai % 2]
            ai += 1
            for c in range(8):
                pgb = pg[ui % 2]
                plb = pl[ui % 2]
                hgb, sgb, hlb = hg[ui % 2], sg[ui % 2], hl[ui % 2]
                ui += 1
                for gl, pb in ((0, pgb), (1, plb)):
                    for kc in range(8):
                        k.op("pe", lambda e, kc=kc, gl=gl, pb=pb, c=c: e.matmul(
                            pb[:, 0:TT],
                            lhsT=gu[kc][:, 2 * 128 * c + gl: 2 * 128 * (c + 1): 2],
                            rhs=XT.t[:, kc, tt * TT:(tt + 1) * TT], start=(kc == 0), stop=(kc == 7)),
                            R=[gu[kc], XT], W=[pb])
                k.op("dve", lambda e, c=c, pgb=pgb, hgb=hgb: e.tensor_scalar(
                    out=hgb[:], in0=pgb[:, 0:TT], scalar1=bguT[:, c, 0, ex:ex + 1], scalar2=7.0,
                    op0=ALU.add, op1=ALU.min), R=[pgb, bguT], W=[hgb])
                k.op("act", lambda e, hgb=hgb, sgb=sgb: e.activation(
                    out=sgb[:], in_=hgb[:], func=AF.Sigmoid, scale=1.702), R=[hgb], W=[sgb])
                k.op("act", lambda e, c=c, plb=plb, hlb=hlb: e.activation(
                    out=hlb[:], in_=plb[:, 0:TT], func=AF.Identity, bias=bguT[:, c, 1, ex:ex + 1], scale=1.0),
                    R=[plb, bguT], W=[hlb])
                k.op("dve", lambda e, hlb=hlb: e.tensor_scalar(
                    out=hlb[:], in0=hlb[:], scalar1=7.0, scalar2=-7.0, op0=ALU.min, op1=ALU.max),
                    R=[hlb], W=[hlb])
                k.op("pool", lambda e, hgb=hgb, sgb=sgb: e.tensor_tensor(
                    out=sgb[:], in0=hgb[:], in1=sgb[:], op=ALU.mult), R=[hgb, sgb], W=[sgb])
                k.op("dve", lambda e, c=c, hlb=hlb, sgb=sgb, at=at: e.scalar_tensor_tensor(
                    out=at[:, c, :], in0=hlb[:], scalar=1.0, in1=sgb[:], op0=ALU.add, op1=ALU.mult),
                    R=[hlb, sgb], W=[at])
            for s in range(NSUB):
                tb = (tt * TT) // 128 + s
                for half in range(2):
                    pb = py[(s * 2 + half) % 2]
                    for c in range(8):
                        k.op("pe", lambda e, c=c, s=s, half=half, pb=pb: e.matmul(
                            pb[:], lhsT=at[:, c, s * 128:(s + 1) * 128],
                            rhs=dn[c][:, half * 512:(half + 1) * 512],
                            start=(c == 0), stop=(c == 7)), R=[at, dn[c]], W=[pb])
                    k.op("dve", lambda e, tb=tb, half=half, pb=pb: e.scalar_tensor_tensor(
                        out=Z[tb][:, half * 512:(half + 1) * 512], in0=pb[:], scalar=G[:, tb, ex:ex + 1],
                        in1=Z[tb][:, half * 512:(half + 1) * 512], op0=ALU.mult, op1=ALU.add),
                        R=[pb, G, Z[tb]], W=[Z[tb]])

    for tb in range(NT):
        if STAGE < 2:
            k.dma("sp", Z[tb][:], h_d.t[tb * 128:(tb + 1) * 128, :], R=[h_d], W=[Z[tb]])
        if STAGE >= 5:
            layer_norm_block(k, Z[tb], Z[tb][:], lng, lnb, lntmp)
        k.dma("sp", out_d.t[tb * 128:(tb + 1) * 128, :], Z[tb][:], R=[Z[tb]], W=[out_d])
    return Z


NEG_BIG = -30000.0


class AttnCtx:
    def __init__(self, k, sc_banks, n_pt=4):
        self.k = k
        self.sc = sc_banks
        self.pt = [k.sb([128, 512], BF16, "pt%d" % i) for i in range(n_pt)]
        self.ui = 0


def attn_unit(ctx, kt_ap, q_ap, Rk, Rq, scale, v_ap, Rv, accs, first, last, masks=(), nq=512,
              bias_mm=None, kparts=128):
    k = ctx.k
    sc = ctx.sc[ctx.ui % len(ctx.sc)]
    pt = ctx.pt[ctx.ui % len(ctx.pt)]
    ctx.ui += 1
    k.op("pe", lambda e: e.matmul(sc[0:kparts, 0:nq], lhsT=kt_ap, rhs=q_ap, start=True,
                                  stop=(bias_mm is None)), R=list(Rk) + list(Rq), W=[sc])
    if bias_mm is not None:
        ind_ap, b_ap, Rb = bias_mm
        k.op("pe", lambda e: e.matmul(sc[0:kparts, 0:nq], lhsT=ind_ap, rhs=b_ap, start=False, stop=True),
             R=list(Rb), W=[sc])
    k.op("act", lambda e: e.activation(out=pt[0:kparts, 0:nq], in_=sc[0:kparts, 0:nq], func=AF.Exp, scale=scale),
         R=[sc], W=[pt])
    for m in masks:
        k.op("pool", lambda e: e.affine_select(out=pt[0:kparts, 0:nq], in_=pt[0:kparts, 0:nq],
                                               pattern=[[m["step"], nq]], compare_op=ALU.is_ge, fill=k.fillreg(0.0),
                                               base=m["base"], channel_multiplier=m["cm"]),
             R=[pt], W=[pt])
    for s, (ab, aap, bank_first) in enumerate(accs):
        k.op("pe", lambda e: e.matmul(aap, lhsT=pt[0:kparts, s * 128:(s + 1) * 128], rhs=v_ap,
                                      start=(first and bank_first), stop=last, skip_group_check=True),
             R=[pt] + list(Rv), W=[ab])


def causal_masks(q0, k0, nq=512, kparts=128):
    if k0 + kparts - 1 <= q0:
        return []
    return [dict(base=q0 - k0, cm=-1, step=1)]


def emit_da_proj(k, T, xT_d, w_d, wsw_d, c2_d, s2_d, QT_d, KT_d, V1_d, psum):
    xT = k.sb([128, 8, T], BF16, "xT")
    k.dma("pool", xT[:], xT_d.t, R=[xT_d], W=[xT])
    w = k.sb([128, 8, 3072], BF16, "w_in")
    wsw = k.sb([128, 8, 2048], BF16, "w_sw")
    for kc in range(8):
        k.dma("pool", w[:, kc, :], w_d.t[kc * 128:(kc + 1) * 128, :], R=[w_d], W=[w])
        k.dma("pool", wsw[:, kc, :], wsw_d.t[kc * 128:(kc + 1) * 128, :], R=[wsw_d], W=[wsw])
    c2 = k.sb([128, T], F32, "c2")
    s2 = k.sb([128, T], F32, "s2")
    k.dma("sp", c2[:], c2_d.t, R=[c2_d], W=[c2])
    k.dma("sp", s2[:], s2_d.t, R=[s2_d], W=[s2])
    TT = min(512, T)
    t1 = [k.sb([128, TT], F32, "rt1_%d" % i) for i in range(2)]
    t2 = [k.sb([128, TT], F32, "rt2_%d" % i) for i in range(2)]
    ob = [k.sb([128, TT], BF16, "rob_%d" % i) for i in range(2)]
    ui = 0
    for which, dst in ((0, QT_d), (1, KT_d)):
        for h in range(8):
            col = which * 1024 + h * 128
            for tt in range(T // TT):
                pa = psum[(ui * 2) % 4]
                pb = psum[(ui * 2 + 1) % 4]
                a1, a2, o = t1[ui % 2], t2[ui % 2], ob[ui % 2]
                ui += 1
                for kc in range(8):
                    k.op("pe", lambda e: e.matmul(pa[:, 0:TT], lhsT=w[:, kc, col:col + 128],
                                                  rhs=xT[:, kc, tt * TT:(tt + 1) * TT], start=(kc == 0), stop=(kc == 7)),
                         R=[w, xT], W=[pa])
                for kc in range(8):
                    k.op("pe", lambda e: e.matmul(pb[:, 0:TT], lhsT=wsw[:, kc, col:col + 128],
                                                  rhs=xT[:, kc, tt * TT:(tt + 1) * TT], start=(kc == 0), stop=(kc == 7)),
                         R=[wsw, xT], W=[pb])
                k.op("dve", lambda e: e.tensor_tensor(out=a1[:], in0=pa[:, 0:TT], in1=c2[:, tt * TT:(tt + 1) * TT],
                                                      op=ALU.mult), R=[pa, c2], W=[a1])
                k.op("dve", lambda e: e.tensor_tensor(out=a2[:], in0=pb[:, 0:TT], in1=s2[:, tt * TT:(tt + 1) * TT],
                                                      op=ALU.mult), R=[pb, s2], W=[a2])
                k.op("pool", lambda e: e.tensor_tensor(out=o[:], in0=a1[:], in1=a2[:], op=ALU.add),
                     R=[a1, a2], W=[o])
                k.dma("sp", dst.t[h, :, tt * TT:(tt + 1) * TT], o[:], R=[o], W=[dst])
    vb = [k.sb([128, 8, 129], BF16, "vb%d" % i) for i in range(2)]
    for b in vb:
        k.op("pool", lambda e: e.memset(b[:], 1.0), W=[b])
    for tb in range(T // 128):
        v = vb[tb % 2]
        for half in range(2):
            pa = psum[4 + half]
            for kc in range(8):
                k.op("pe", lambda e: e.matmul(pa[:], lhsT=xT[:, kc, tb * 128:(tb + 1) * 128],
                                              rhs=w[:, kc, 2048 + half * 512:2048 + (half + 1) * 512],
                                              start=(kc == 0), stop=(kc == 7)), R=[xT, w], W=[pa])
            k.op("act", lambda e: e.activation(out=v[:, half * 4:(half + 1) * 4, 0:128],
                                               in_=pa[:].rearrange("p (h d) -> p h d", h=4), func=AF.Copy),
                 R=[pa], W=[v])
        k.dma("sp", V1_d.t[tb * 128:(tb + 1) * 128, :, :], v[:], R=[v], W=[V1_d])


def emit_da_attn(k, S, QT_d, KT_d, V1_d, lq1_d, lk1_d, lq2_d, lk2_d, g_d, O_d, lam_init, psum):
    NKT = S // 128
    NQ = min(512, S)
    NQT = S // NQ
    NS = NQ // 128
    QT = k.sb([128, S], BF16, "QT")
    KT = k.sb([128, S], BF16, "KT")
    V1 = k.sb([128, NKT, 129], BF16, "V1")
    nchunk = 4 if S >= 2048 else 1
    cs = S // nchunk
    for i in range(nchunk):
        k.dma("sp", KT[:, i * cs:(i + 1) * cs], KT_d.t[:, i * cs:(i + 1) * cs], R=[KT_d], W=[KT])
        k.dma("sp", V1[:, i * (NKT // nchunk):(i + 1) * (NKT // nchunk), :],
              V1_d.t[:, i * (NKT // nchunk):(i + 1) * (NKT // nchunk), :], R=[V1_d], W=[V1])
        k.dma("sp", QT[:, i * cs:(i + 1) * cs], QT_d.t[:, i * cs:(i + 1) * cs], R=[QT_d], W=[QT])
    lam4 = k.sb([128, 4, 64], F32, "lam4")
    for i, d in enumerate((lq1_d, lk1_d, lq2_d, lk2_d)):
        k.dma("sp", lam4[:, i, :], d.t.partition_broadcast(128), R=[d], W=[lam4])
    lprod = k.sb([128, 2, 64], F32, "lprod")
    lsum = k.sb([128, 2], F32, "lsum")
    nlam = k.sb([128, 1], F32, "nlam")
    k.op("dve", lambda e: e.tensor_tensor(out=lprod[:, 0, :], in0=lam4[:, 0, :], in1=lam4[:, 1, :], op=ALU.mult),
         R=[lam4], W=[lprod])
    k.op("dve", lambda e: e.tensor_tensor(out=lprod[:, 1, :], in0=lam4[:, 2, :], in1=lam4[:, 3, :], op=ALU.mult),
         R=[lam4], W=[lprod])
    k.op("dve", lambda e: e.reduce_sum(out=lsum[:], in_=lprod[:], axis=AX.X), R=[lprod], W=[lsum])
    k.op("act", lambda e: e.activation(out=lsum[:], in_=lsum[:], func=AF.Exp), R=[lsum], W=[lsum])
    k.op("dve", lambda e: e.tensor_tensor(out=nlam[:], in0=lsum[:, 1:2], in1=lsum[:, 0:1], op=ALU.subtract),
         R=[lsum], W=[nlam])
    k.op("dve", lambda e: e.tensor_scalar(out=nlam[:], in0=nlam[:], scalar1=-lam_init, scalar2=None, op0=ALU.add),
         R=[nlam], W=[nlam])
    gsc = k.sb([128, 128], F32, "gsc")
    k.dma("sp", gsc[:], g_d.t.partition_broadcast(128), R=[g_d], W=[gsc])
    k.op("dve", lambda e: e.tensor_scalar(out=gsc[:], in0=gsc[:], scalar1=(1.0 - lam_init), scalar2=None,
                                          op0=ALU.mult), R=[gsc], W=[gsc])
    eps = k.sb([128, 1], F32, "eps_da")
    k.op("pool", lambda e: e.memset(eps[:], 1e-5), W=[eps])

    ctx = AttnCtx(k, psum[0:4])
    accb = psum[4:7]
    def acc_of(half, s):
        if s < 3:
            b = accb[half]
            return (b, b[:, s * 129:(s + 1) * 129], s == 0)
        b = accb[2]
        return (b, b[:, half * 129:(half + 1) * 129], half == 0)

    r1 = k.sb([128, 1], F32, "r1")
    r2 = k.sb([128, 1], F32, "r2")
    o1 = k.sb([128, 128], F32, "o1")
    oo = k.sb([128, 128], F32, "oo")
    sq = k.sb([128, 128], F32, "sq")
    ss = k.sb([128, 1], F32, "ss")
    ob = [k.sb([128, 128], BF16, "ob%d" % i) for i in range(2)]
    oi = 0
    scale = 64.0 ** -0.5
    for qt in range(NQT):
        q0 = qt * NQ
        nk = (q0 + NQ) // 128
        for half in range(2):
            accs = [acc_of(half, s) for s in range(NS)]
            pr = slice(half * 64, (half + 1) * 64)
            for kt in range(nk):
                attn_unit(ctx, KT[pr, kt * 128:(kt + 1) * 128], QT[pr, q0:q0 + NQ], [KT], [QT], scale,
                          V1[:, kt, :], [V1], accs, first=(kt == 0), last=(kt == nk - 1),
                          masks=causal_masks(q0, kt * 128, NQ), nq=NQ)
        for s in range(NS):
            b0, a0, _ = acc_of(0, s)
            b1, a1, _ = acc_of(1, s)
            k.op("dve", lambda e: e.reciprocal(out=r1[:], in_=a0[:, 128:129]), R=[b0], W=[r1])
            k.op("dve", lambda e: e.reciprocal(out=r2[:], in_=a1[:, 128:129]), R=[b1], W=[r2])
            k.op("dve", lambda e: e.tensor_tensor(out=r2[:], in0=r2[:], in1=nlam[:], op=ALU.mult),
                 R=[r2, nlam], W=[r2])
            k.op("act", lambda e: e.activation(out=o1[:], in_=a0[:, 0:128], func=AF.Copy, scale=r1[:]),
                 R=[b0, r1], W=[o1])
            k.op("dve", lambda e: e.scalar_tensor_tensor(out=oo[:], in0=a1[:, 0:128], scalar=r2[:], in1=o1[:],
                                                         op0=ALU.mult, op1=ALU.add), R=[b1, r2, o1], W=[oo])
            k.op("act", lambda e: e.activation(out=sq[:], in_=oo[:], func=AF.Square, accum_out=ss[:]),
                 R=[oo], W=[sq, ss])
            k.op("act", lambda e: e.activation(out=ss[:], in_=ss[:], func=AF.Sqrt, scale=1.0 / 128.0, bias=eps[:]),
                 R=[ss, eps], W=[ss])
            k.op("dve", lambda e: e.reciprocal(out=ss[:], in_=ss[:]), R=[ss], W=[ss])
            o = ob[oi % 2]
            oi += 1
            k.op("dve", lambda e: e.scalar_tensor_tensor(out=o[:], in0=oo[:], scalar=ss[:], in1=gsc[:],
                                                         op0=ALU.mult, op1=ALU.mult), R=[oo, ss, gsc], W=[o])
            k.dma("sp", O_d.t[q0 + s * 128:q0 + (s + 1) * 128, :], o[:], R=[o], W=[O_d])


def load_w_bf(k, w_d, K, N, name, col0=0):
    if K >= 128:
        KC = K // 128
        w = k.sb([128, KC, N], BF16, name)
        for kc in range(KC):
            k.dma("pool", w[:, kc, :], w_d.t[kc * 128:(kc + 1) * 128, col0:col0 + N], R=[w_d], W=[w])
    else:
        w = k.sb([K, 1, N], BF16, name)
        k.dma("pool", w[:, 0, :], w_d.t[:, col0:col0 + N], R=[w_d], W=[w])
    return w


class ProjCtx:
    def __init__(self, k, psum, TT):
        self.k = k
        self.psum = psum
        self.TT = TT
        self.t1 = [k.sb([128, TT], F32, "pj1_%d" % i) for i in range(2)]
        self.t2 = [k.sb([128, TT], F32, "pj2_%d" % i) for i in range(2)]
        self.ob = [k.sb([128, TT], BF16, "pjo_%d" % i) for i in range(3)]
        self.ui = 0


def proj_fm(pc, xT, KC, T, w, col, ncols, dst, dst_fn, rope=None, prow=0):
    k = pc.k
    TT = pc.TT
    pr = slice(prow, prow + ncols)
    for tt in range(T // TT):
        ts = slice(tt * TT, (tt + 1) * TT)
        pa = pc.psum[(pc.ui * 2) % 4]
        pb = pc.psum[(pc.ui * 2 + 1) % 4]
        a1, a2, o = pc.t1[pc.ui % 2], pc.t2[pc.ui % 2], pc.ob[pc.ui % 3]
        pc.ui += 1
        for kc in range(KC):
            k.op("pe", lambda e: e.matmul(pa[pr, 0:TT], lhsT=w[:, kc, col:col + ncols], rhs=xT[:, kc, ts],
                                          start=(kc == 0), stop=(kc == KC - 1)), R=[w, xT], W=[pa])
        if rope is None:
            k.op("act", lambda e: e.activation(out=o[pr, :], in_=pa[pr, 0:TT], func=AF.Copy), R=[pa], W=[o])
        else:
            wsw, col_sw, c2, s2 = rope
            for kc in range(KC):
                k.op("pe", lambda e: e.matmul(pb[pr, 0:TT], lhsT=wsw[:, kc, col_sw:col_sw + ncols], rhs=xT[:, kc, ts],
                                              start=(kc == 0), stop=(kc == KC - 1)), R=[wsw, xT], W=[pb])
            k.op("dve", lambda e: e.tensor_tensor(out=a1[pr, :], in0=pa[pr, 0:TT], in1=c2[pr, ts], op=ALU.mult),
                 R=[pa, c2], W=[a1])
            k.op("dve", lambda e: e.tensor_tensor(out=a2[pr, :], in0=pb[pr, 0:TT], in1=s2[pr, ts], op=ALU.mult),
                 R=[pb, s2], W=[a2])
            k.op("pool", lambda e: e.tensor_tensor(out=o[pr, :], in0=a1[pr, :], in1=a2[pr, :], op=ALU.add),
                 R=[a1, a2], W=[o])
        k.dma("sp", dst_fn(ts), o[pr, :], R=[o], W=[dst])


def proj_tm_v1(k, psum, xT, KC, T, w, wview_fn, nh, dv, dst):
    vb = [k.sb([128, nh, dv + 1], BF16, "v1b%d" % i) for i in range(2)]
    for b in vb:
        k.op("pool", lambda e: e.memset(b[:], 1.0), W=[b])
    for tb in range(T // 128):
        v = vb[tb % 2]
        pa = psum[4 + tb % 2]
        for kc in range(KC):
            k.op("pe", lambda e: e.matmul(pa[:, 0:nh * dv], lhsT=xT[:, kc, tb * 128:(tb + 1) * 128], rhs=wview_fn(kc),
                                          start=(kc == 0), stop=(kc == KC - 1)), R=[xT, w], W=[pa])
        k.op("act", lambda e: e.activation(out=v[:, :, 0:dv],
                                           in_=pa[:, 0:nh * dv].rearrange("p (h d) -> p h d", h=nh), func=AF.Copy),
             R=[pa], W=[v])
        k.dma("sp", dst.t[tb * 128:(tb + 1) * 128, :, :], v[:], R=[v], W=[dst])


def causal_attn_head(k, ctx, S, dk, dv, QT, KT, V1, scale, accb, O_d, ocol, fin):
    NQ = min(512, S)
    NS = NQ // 128
    dv1 = dv + 1
    r1, ob = fin
    for qt in range(S // NQ):
        q0 = qt * NQ
        nk = (q0 + NQ) // 128
        accs = [(accb, accb[:, s * dv1:(s + 1) * dv1], s == 0) for s in range(NS)]
        for kt in range(nk):
            attn_unit(ctx, KT[0:dk, kt * 128:(kt + 1) * 128], QT[0:dk, q0:q0 + NQ], [KT], [QT], scale,
                      V1[:, kt, :], [V1], accs, first=(kt == 0), last=(kt == nk - 1),
                      masks=causal_masks(q0, kt * 128, NQ), nq=NQ)
        o = ob[qt % 2]
        for s in range(NS):
            a = accb[:, s * dv1:(s + 1) * dv1]
            k.op("dve", lambda e: e.reciprocal(out=r1[:, s:s + 1], in_=a[:, dv:dv1]), R=[accb], W=[r1])
            k.op("act", lambda e: e.activation(out=o[:, s, :], in_=a[:, 0:dv], func=AF.Copy, scale=r1[:, s:s + 1]),
                 R=[accb, r1], W=[o])
        k.dma("sp", O_d.t[q0:q0 + NQ, ocol:ocol + dv].rearrange("(s p) d -> p s d", p=128), o[:, 0:NS, :],
              R=[o], W=[O_d])


RMS_EPS = 1e-6


def emit_mla_proj(k, T, xT_d, win_d, qn_d, kvn_d, wuq_d, wuqsw_d, wukv_d, cs16_d, c2_d, s2_d,
                  QT_d, KnT_d, KrT_d, V1_d, psum):
    xT = k.sb([128, 8, T], BF16, "xT")
    k.dma("pool", xT[:], xT_d.t, R=[xT_d], W=[xT])
    win = load_w_bf(k, win_d, 1024, 416, "win")
    wuq = load_w_bf(k, wuq_d, 256, 1536, "wuq")
    wuqsw = load_w_bf(k, wuqsw_d, 256, 512, "wuqsw")
    wukv = load_w_bf(k, wukv_d, 128, 2048, "wukv")
    qn = k.sb([128, 256], F32, "qn")
    kvn = k.sb([128, 128], F32, "kvn")
    k.dma("sp", qn[:], qn_d.t.partition_broadcast(128), R=[qn_d], W=[qn])
    k.dma("sp", kvn[:], kvn_d.t.partition_broadcast(128), R=[kvn_d], W=[kvn])
    c2 = k.sb([128, T], F32, "c2")
    s2 = k.sb([128, T], F32, "s2")
    k.dma("sp", c2[:], c2_d.t, R=[c2_d], W=[c2])
    k.dma("sp", s2[:], s2_d.t, R=[s2_d], W=[s2])
    cs16 = k.sb([128, T // 128, 2, 16], F32, "cs16")
    k.dma("sp", cs16[:], cs16_d.t.rearrange("(b p) a i -> p b a i", p=128), R=[cs16_d], W=[cs16])
    ident_f = make_ident(k, F32, "ident_f")
    eps = k.sb([128, 1], F32, "eps_rms")
    k.op("pool", lambda e: e.memset(eps[:], RMS_EPS), W=[eps])
    cqT = k.sb([128, 2, T], BF16, "cqT")
    ckvT = k.sb([128, 1, T], BF16, "ckvT")
    krT = k.sb([32, T], BF16, "krT")
    c_sb = k.sb([128, 416], F32, "c_sb")
    sq = k.sb([128, 256], F32, "sq")
    ss = k.sb([128, 2], F32, "ss")
    kr = k.sb([128, 32], F32, "kr")
    ktmp = k.sb([128, 2, 16], F32, "ktmp")
    for tb in range(T // 128):
        pa = psum[4]
        for kc in range(8):
            k.op("pe", lambda e: e.matmul(pa[:, 0:416], lhsT=xT[:, kc, tb * 128:(tb + 1) * 128], rhs=win[:, kc, :],
                                          start=(kc == 0), stop=(kc == 7)), R=[xT, win], W=[pa])
        k.op("act", lambda e: e.activation(out=c_sb[:], in_=pa[:, 0:416], func=AF.Copy), R=[pa], W=[c_sb])
        k.op("act", lambda e: e.activation(out=sq[:, 0:256], in_=c_sb[:, 0:256], func=AF.Square, accum_out=ss[:, 0:1]),
             R=[c_sb], W=[sq, ss])
        k.op("act", lambda e: e.activation(out=sq[:, 0:128], in_=c_sb[:, 256:384], func=AF.Square, accum_out=ss[:, 1:2]),
             R=[c_sb, sq, ss], W=[sq, ss])
        k.op("act", lambda e: e.activation(out=ss[:, 0:1], in_=ss[:, 0:1], func=AF.Sqrt, scale=1.0 / 256, bias=eps[:]),
             R=[ss, eps], W=[ss])
        k.op("act", lambda e: e.activation(out=ss[:, 1:2], in_=ss[:, 1:2], func=AF.Sqrt, scale=1.0 / 128, bias=eps[:]),
             R=[ss, eps], W=[ss])
        k.op("dve", lambda e: e.reciprocal(out=ss[:], in_=ss[:]), R=[ss], W=[ss])
        k.op("dve", lambda e: e.scalar_tensor_tensor(out=c_sb[:, 0:256], in0=c_sb[:, 0:256], scalar=ss[:, 0:1],
                                                     in1=qn[:], op0=ALU.mult, op1=ALU.mult), R=[c_sb, ss, qn], W=[c_sb])
        k.op("dve", lambda e: e.scalar_tensor_tensor(out=c_sb[:, 256:384], in0=c_sb[:, 256:384], scalar=ss[:, 1:2],
                                                     in1=kvn[:], op0=ALU.mult, op1=ALU.mult), R=[c_sb, ss, kvn], W=[c_sb])
        cc = cs16[:, tb, 0, :]
        sn = cs16[:, tb, 1, :]
        k.op("dve", lambda e: e.tensor_tensor(out=ktmp[:, 0, :], in0=c_sb[:, 384:400], in1=cc, op=ALU.mult),
             R=[c_sb, cs16], W=[ktmp])
        k.op("dve", lambda e: e.tensor_tensor(out=ktmp[:, 1, :], in0=c_sb[:, 400:416], in1=sn, op=ALU.mult),
             R=[c_sb, cs16, ktmp], W=[ktmp])
        k.op("dve", lambda e: e.tensor_tensor(out=kr[:, 0:16], in0=ktmp[:, 0, :], in1=ktmp[:, 1, :], op=ALU.subtract),
             R=[ktmp], W=[kr])
        k.op("dve", lambda e: e.tensor_tensor(out=ktmp[:, 0, :], in0=c_sb[:, 400:416], in1=cc, op=ALU.mult),
             R=[c_sb, cs16, kr], W=[ktmp])
        k.op("dve", lambda e: e.tensor_tensor(out=ktmp[:, 1, :], in0=c_sb[:, 384:400], in1=sn, op=ALU.mult),
             R=[c_sb, cs16, ktmp], W=[ktmp])
        k.op("dve", lambda e: e.tensor_tensor(out=kr[:, 16:32], in0=ktmp[:, 0, :], in1=ktmp[:, 1, :], op=ALU.add),
             R=[ktmp, kr], W=[kr])
        pt = psum[5]
        for j in range(3):
            k.op("pe", lambda e: e.transpose(out=pt[:, j * 128:(j + 1) * 128], in_=c_sb[:, j * 128:(j + 1) * 128],
                                             identity=ident_f[:]), R=[c_sb, ident_f], W=[pt])
        k.op("pe", lambda e: e.transpose(out=pt[0:32, 384:512], in_=kr[:], identity=ident_f[:]),
             R=[kr, ident_f], W=[pt])
        k.op("act", lambda e: e.activation(out=cqT[:, :, tb * 128:(tb + 1) * 128],
                                           in_=pt[:, 0:256].rearrange("p (j t) -> p j t", j=2), func=AF.Copy),
             R=[pt], W=[cqT])
        k.op("dve", lambda e: e.tensor_copy(out=ckvT[:, 0, tb * 128:(tb + 1) * 128], in_=pt[:, 256:384]),
             R=[pt], W=[ckvT])
        k.op("dve", lambda e: e.tensor_copy(out=krT[:, tb * 128:(tb + 1) * 128], in_=pt[0:32, 384:512]),
             R=[pt], W=[krT])
    k.dma("sp", KrT_d.t, krT[:], R=[krT], W=[KrT_d])
    pc = ProjCtx(k, psum, min(512, T))
    for h in range(16):
        proj_fm(pc, cqT, 2, T, wuq, h * 96, 64, QT_d, lambda ts: QT_d.t[h, 0:64, ts])
        proj_fm(pc, cqT, 2, T, wuq, h * 96 + 64, 32, QT_d, lambda ts: QT_d.t[h, 64:96, ts],
                rope=(wuqsw, h * 32, c2, s2), prow=64)
        proj_fm(pc, ckvT, 1, T, wukv, h * 128, 64, KnT_d, lambda ts: KnT_d.t[h, :, ts])
    wv = wukv[:, 0, :].rearrange("p (h d) -> p h d", h=16)
    for half in range(2):
        pass
    vb = [k.sb([128, 16, 65], BF16, "v1b%d" % i) for i in range(2)]
    for b in vb:
        k.op("pool", lambda e: e.memset(b[:], 1.0), W=[b])
    for tb in range(T // 128):
        v = vb[tb % 2]
        for half in range(2):
            pa = psum[4 + half]
            k.op("pe", lambda e: e.matmul(pa[:], lhsT=ckvT[:, 0, tb * 128:(tb + 1) * 128],
                                          rhs=wv[:, half * 8:(half + 1) * 8, 64:128], start=True, stop=True),
                 R=[ckvT, wukv], W=[pa])
            k.op("act", lambda e: e.activation(out=v[:, half * 8:(half + 1) * 8, 0:64],
                                               in_=pa[:].rearrange("p (h d) -> p h d", h=8), func=AF.Copy),
                 R=[pa], W=[v])
        k.dma("sp", V1_d.t[tb * 128:(tb + 1) * 128, :, :], v[:], R=[v], W=[V1_d])


class ckvT_view:
    def __init__(self, b):
        self.b = b
        self.lw = None

    def __getitem__(self, idx):
        p, kc, ts = idx
        return self.b[p, ts]


def emit_mla_attn(k, S, QT_d, KT_d, V1_d, O_d, psum, nheads=2):
    NKT = S // 128
    ctx = AttnCtx(k, psum[0:4])
    r1 = k.sb([128, 4], F32, "r1")
    ob = [k.sb([128, 4, 64], BF16, "ob%d" % i) for i in range(2)]
    QT = [k.sb([96, S], BF16, "QT%d" % i) for i in range(2)]
    KT = [k.sb([96, S], BF16, "KT%d" % i) for i in range(2)]
    V1 = [k.sb([128, NKT, 65], BF16, "V1%d" % i) for i in range(2)]
    scale = 96.0 ** -0.5
    for h in range(nheads):
        q, kk, v = QT[h % 2], KT[h % 2], V1[h % 2]
        k.dma("sp", kk[:], KT_d.t[h], R=[KT_d], W=[kk])
        k.dma("sp", v[:], V1_d.t[h], R=[V1_d], W=[v])
        k.dma("sp", q[:], QT_d.t[h], R=[QT_d], W=[q])
        causal_attn_head(k, ctx, S, 96, 64, q, kk, v, scale, psum[4 + h % 2], O_d, h * 64, (r1, ob))


def emit_nsa_proj(k, T, xT_d, w_d, wsw_d, c2_d, s2_d, QT_d, KsT_d, KwT_d, KcT_d, VcT_d, V1s_d, V1w_d, gate_d, psum):
    xT = k.sb([128, 8, T], BF16, "xT")
    k.dma("pool", xT[:], xT_d.t, R=[xT_d], W=[xT])
    w = load_w_bf(k, w_d, 1024, 2608, "w_in")
    wsw = load_w_bf(k, wsw_d, 1024, 1536, "w_sw")
    c2 = k.sb([128, T], F32, "c2")
    s2 = k.sb([128, T], F32, "s2")
    k.dma("sp", c2[:], c2_d.t, R=[c2_d], W=[c2])
    k.dma("sp", s2[:], s2_d.t, R=[s2_d], W=[s2])
    pc = ProjCtx(k, psum, min(512, T))
    for h in range(8):
        proj_fm(pc, xT, 8, T, w, h * 128, 128, QT_d, lambda ts: QT_d.t[h, :, ts], rope=(wsw, h * 128, c2, s2))
    for j in range(2):
        proj_fm(pc, xT, 8, T, w, 1536 + j * 128, 128, KsT_d, lambda ts: KsT_d.t[j, :, ts],
                rope=(wsw, 1024 + j * 128, c2, s2))
        proj_fm(pc, xT, 8, T, w, 2048 + j * 128, 128, KwT_d, lambda ts: KwT_d.t[j, :, ts],
                rope=(wsw, 1280 + j * 128, c2, s2))
        proj_fm(pc, xT, 8, T, w, 1024 + j * 128, 128, KcT_d, lambda ts: KcT_d.t[j, :, ts])
        proj_fm(pc, xT, 8, T, w, 1280 + j * 128, 128, VcT_d, lambda ts: VcT_d.t[j, :, ts])
    proj_tm_v1(k, psum, xT, 8, T, w, lambda kc: w[:, kc, 1792:2048], 4, 64, V1s_d)
    proj_tm_v1(k, psum, xT, 8, T, w, lambda kc: w[:, kc, 2304:2560], 4, 64, V1w_d)
    gt = [k.sb([128, 48], F32, "gt%d" % i) for i in range(2)]
    for tb in range(T // 128):
        pa = psum[6 + tb % 2]
        g = gt[tb % 2]
        for kc in range(8):
            k.op("pe", lambda e: e.matmul(pa[:, 0:48], lhsT=xT[:, kc, tb * 128:(tb + 1) * 128], rhs=w[:, kc, 2560:2608],
                                          start=(kc == 0), stop=(kc == 7)), R=[xT, w], W=[pa])
        k.op("act", lambda e: e.activation(out=g[:], in_=pa[:, 0:48], func=AF.Sigmoid), R=[pa], W=[g])
        k.dma("sp", gate_d.t[tb * 128:(tb + 1) * 128, :], g[:], R=[g], W=[gate_d])


def emit_nsa_compress(k, NB, win_k_d, win_v_d, posk_d, posv_d, w1k_d, w1v_d, w2k_d, w2ksw_d, w2v_d, c2_d, s2_d,
                      KcT_d, V1c_d, psum):
    W = 16 * NB + 16
    ident_f = make_ident(k, F32, "ident_f")
    c2 = k.sb([64, NB], F32, "c2c")
    s2 = k.sb([64, NB], F32, "s2c")
    k.dma("sp", c2[:], c2_d.t, R=[c2_d], W=[c2])
    k.dma("sp", s2[:], s2_d.t, R=[s2_d], W=[s2])
    v1 = k.sb([128, 65], BF16, "v1c")
    k.op("pool", lambda e: e.memset(v1[:], 1.0), W=[v1])
    for kv, (win_d, pos_d, w1_d, w2_d) in enumerate(((win_k_d, posk_d, w1k_d, w2k_d), (win_v_d, posv_d, w1v_d, w2v_d))):
        win = k.sb([64, 4, W], BF16, "win%d" % kv)
        k.dma("sp", win[:], win_d.t.rearrange("g d w -> d g w"), R=[win_d], W=[win])
        w1 = k.sb([64, 32, 256], BF16, "w1_%d" % kv)
        k.dma("pool", w1[:], w1_d.t.rearrange("(l d) h -> d l h", d=64), R=[w1_d], W=[w1])
        w2 = k.sb([128, 2, 64], BF16, "w2_%d" % kv)
        k.dma("pool", w2[:], w2_d.t.rearrange("(c p) d -> p c d", p=128), R=[w2_d], W=[w2])
        if kv == 0:
            w2sw = k.sb([128, 2, 64], BF16, "w2sw")
            k.dma("pool", w2sw[:], w2ksw_d.t.rearrange("(c p) d -> p c d", p=128), R=[w2ksw_d], W=[w2sw])
        pos = k.sb([32, 64], F32, "pos%d" % kv)
        k.dma("sp", pos[:], pos_d.t, R=[pos_d], W=[pos])
        posT = k.sb([64, 32], BF16, "posT%d" % kv)
        pm = psum[7]
        k.op("pe", lambda e: e.transpose(out=pm[0:64, 0:32], in_=pos[:], identity=ident_f[0:32, 0:32]),
             R=[pos, ident_f], W=[pm])
        k.op("act", lambda e: e.activation(out=posT[:], in_=pm[0:64, 0:32], func=AF.Copy), R=[pm], W=[posT])
        pbias = k.sb([128, 2], F32, "pbias%d" % kv)
        for hc in range(2):
            for l in range(32):
                k.op("pe", lambda e: e.matmul(pm[:, 64 + hc:65 + hc], lhsT=w1[:, l, hc * 128:(hc + 1) * 128],
                                              rhs=posT[:, l:l + 1], start=(l == 0), stop=(l == 31)),
                     R=[w1, posT], W=[pm])
        k.op("dve", lambda e: e.tensor_copy(out=pbias[:], in_=pm[:, 64:66]), R=[pm], W=[pbias])
        x = k.sb([128, NB], F32, "gx%d" % kv)
        x2 = k.sb([128, NB], F32, "gx2%d" % kv)
        hT = [k.sb([128, NB], BF16, "hT%d_%d" % (kv, i)) for i in range(2)]
        a1 = k.sb([64, NB], F32, "ca1_%d" % kv)
        a2 = k.sb([64, NB], F32, "ca2_%d" % kv)
        ko = k.sb([64, NB], BF16, "ko%d" % kv)
        for g in range(4):
            for hc in range(2):
                ph = psum[hc]
                for l in range(32):
                    k.op("pe", lambda e: e.matmul(ph[:, 0:NB], lhsT=w1[:, l, hc * 128:(hc + 1) * 128],
                                                  rhs=win[:, g, l:l + 16 * (NB - 1) + 1:16], start=(l == 0), stop=(l == 31)),
                         R=[w1, win], W=[ph])
                k.op("act", lambda e: e.activation(out=x[:], in_=ph[:, 0:NB], func=AF.Identity,
                                                   bias=pbias[:, hc:hc + 1], scale=1.0), R=[ph, pbias], W=[x])
                k.op("dve", lambda e: e.tensor_tensor(out=x2[:], in0=x[:], in1=x[:], op=ALU.mult), R=[x], W=[x2])
                k.op("dve", lambda e: e.tensor_scalar(out=x2[:], in0=x2[:], scalar1=0.044715, scalar2=1.0,
                                                      op0=ALU.mult, op1=ALU.add), R=[x2], W=[x2])
                k.op("dve", lambda e: e.tensor_tensor(out=x2[:], in0=x2[:], in1=x[:], op=ALU.mult), R=[x2, x], W=[x2])
                k.op("act", lambda e: e.activation(out=x2[:], in_=x2[:], func=AF.Sigmoid, scale=1.5957691216057308),
                     R=[x2], W=[x2])
                k.op("dve", lambda e: e.tensor_tensor(out=hT[hc][:], in0=x[:], in1=x2[:], op=ALU.mult),
                     R=[x, x2], W=[hT[hc]])
            if kv == 0:
                pa, pb = psum[2], psum[3]
                for hc in range(2):
                    k.op("pe", lambda e: e.matmul(pa[0:64, 0:NB], lhsT=w2[:, hc, :], rhs=hT[hc][:],
                                                  start=(hc == 0), stop=(hc == 1)), R=[w2, hT[hc]], W=[pa])
                for hc in range(2):
                    k.op("pe", lambda e: e.matmul(pb[0:64, 0:NB], lhsT=w2sw[:, hc, :], rhs=hT[hc][:],
                                                  start=(hc == 0), stop=(hc == 1)), R=[w2sw, hT[hc]], W=[pb])
                k.op("dve", lambda e: e.tensor_tensor(out=a1[:], in0=pa[0:64, 0:NB], in1=c2[:], op=ALU.mult),
                     R=[pa, c2], W=[a1])
                k.op("dve", lambda e: e.tensor_tensor(out=a2[:], in0=pb[0:64, 0:NB], in1=s2[:], op=ALU.mult),
                     R=[pb, s2], W=[a2])
                k.op("dve", lambda e: e.tensor_tensor(out=ko[:], in0=a1[:], in1=a2[:], op=ALU.add), R=[a1, a2], W=[ko])
                k.dma("sp", KcT_d.t[g], ko[:], R=[ko], W=[KcT_d])
            else:
                pa = psum[2]
                for hc in range(2):
                    k.op("pe", lambda e: e.matmul(pa[0:NB, 0:64], lhsT=hT[hc][:], rhs=w2[:, hc, :],
                                                  start=(hc == 0), stop=(hc == 1)), R=[w2, hT[hc]], W=[pa])
                k.op("act", lambda e: e.activation(out=v1[0:NB, 0:64], in_=pa[0:NB, 0:64], func=AF.Copy), R=[pa], W=[v1])
                k.dma("sp", V1c_d.t[g], v1[0:NB, :], R=[v1], W=[V1c_d])


def emit_nsa_attn(k, S, QT4_d, Qdup_d, KcT2_d, V1cA_d, KsKw_d, V1s_d, V1w_d, gate_d, O_d, oc_d, psum):
    NQ = min(512, S)
    NQT = S // NQ
    NS = NQ // 128
    NKT = S // 128
    NSLC = S // 64
    NJC = (NSLC + 127) // 128
    JW = min(128, NSLC)
    NC = ((S - 32) // 16 + 1 + 127) // 128 * 128
    NNT = NC // 128
    CW = 65 + NSLC
    scale = 0.125
    ident_b = make_ident(k, BF16, "ident_b")
    biasT = k.sb([128, NJC, S], BF16, "biasT")
    KcT2 = k.sb([128, NC], BF16, "KcT2")
    k.dma("sp", KcT2[:], KcT2_d.t, R=[KcT2_d], W=[KcT2])
    V1cA = k.sb([128, NNT, CW], BF16, "V1cA")
    k.dma("sp", V1cA[:], V1cA_d.t, R=[V1cA_d], W=[V1cA])
    gate = k.sb([128, NKT, 2, 3], F32, "gate")
    k.dma("sp", gate[:], gate_d.t, R=[gate_d], W=[gate])
    ctx = AttnCtx(k, psum[0:2])
    accb = psum[2:6]
    pmisc = psum[6]
    ptr = psum[7]
    oct = [k.sb([128, 4, 2, 64], BF16, "oct%d" % i) for i in range(2)]
    tiny = 1e-30
    k.push_scope()
    QT4 = k.sb([128, 2, S], BF16, "QT4")
    for p in range(2):
        k.dma("sp", QT4[:, p, :], QT4_d.t[p], R=[QT4_d], W=[QT4])
    imp = k.sb([128, 4, NSLC], F32, "imp")
    rl = k.sb([128, 1], F32, "rl")
    m8a = k.sb([128, 8], F32, "m8a")
    m8b = k.sb([128, 8], F32, "m8b")
    sc2 = k.sb([128, NSLC], F32, "sc2")
    bsel = k.sb([128, NJC * 128], BF16, "bsel")
    if NSLC < 128:
        k.op("pool", lambda e: e.memset(bsel[:], 0.0), W=[bsel])
    for qt in range(NQT):
        q0 = qt * NQ
        nts = [nt for nt in range(NNT) if 16 * (128 * nt) + 31 <= q0 + NQ - 1]
        oc = oct[qt % 2]
        for r in range(4):
            pr = slice((r % 2) * 64, (r % 2) * 64 + 64)
            accs = [(accb[s], accb[s][:, 0:CW], True) for s in range(NS)]
            if not nts:
                pass
            for i, nt in enumerate(nts):
                n0 = nt * 128
                fully = (16 * (n0 + 127) + 31 <= q0)
                masks = [] if fully else [dict(base=q0 - 16 * n0 - 31, cm=-16, step=1)]
                attn_unit(ctx, KcT2[pr, n0:n0 + 128], QT4[pr, r // 2, q0:q0 + NQ], [KcT2], [QT4], scale,
                          V1cA[:, nt, :], [V1cA], accs, first=(i == 0), last=(i == len(nts) - 1), masks=masks, nq=NQ)
            for s in range(NS):
                a = accb[s]
                if nts:
                    k.op("dve", lambda e: e.tensor_scalar(out=rl[:], in0=a[:, 64:65], scalar1=tiny, scalar2=None,
                                                          op0=ALU.max), R=[a], W=[rl])
                    k.op("dve", lambda e: e.reciprocal(out=rl[:], in_=rl[:]), R=[rl], W=[rl])
                    if r == 0:
                        k.op("dve", lambda e: e.tensor_scalar(out=imp[:, s, :], in0=a[:, 65:CW], scalar1=rl[:],
                                                              scalar2=None, op0=ALU.mult), R=[a, rl], W=[imp])
                    else:
                        k.op("dve", lambda e: e.scalar_tensor_tensor(out=imp[:, s, :], in0=a[:, 65:CW], scalar=rl[:],
                                                                     in1=imp[:, s, :], op0=ALU.mult, op1=ALU.add),
                             R=[a, rl, imp], W=[imp])
                    if r < 2:
                        k.op("act", lambda e: e.activation(out=oc[:, s, r, :], in_=a[:, 0:64], func=AF.Copy,
                                                           scale=rl[:]), R=[a, rl], W=[oc])
                else:
                    if r == 0:
                        k.op("pool", lambda e: e.memset(imp[:, s, :], 0.0), W=[imp])
                    if r < 2:
                        k.op("pool", lambda e: e.memset(oc[:, s, r, :], 0.0), W=[oc])
        k.dma("sp", oc_d.t[q0:q0 + NQ, :].rearrange("(s p) (r d) -> p s r d", p=128, r=2), oc[:, 0:NS, :, :],
              R=[oc], W=[oc_d])
        for s in range(NS):
            qb = q0 + s * 128
            sc = imp[:, s, :]
            k.op("pool", lambda e: e.affine_select(out=sc, in_=sc, pattern=[[-64, NSLC]], compare_op=ALU.is_ge,
                                                   fill=k.fillreg(1e9), base=qb - 128, channel_multiplier=1), R=[imp], W=[imp])
            k.op("pool", lambda e: e.memset(imp[:, s, 0:1], 1e9), R=[imp], W=[imp])
            k.op("pool", lambda e: e.affine_select(out=sc, in_=sc, pattern=[[-64, NSLC]], compare_op=ALU.is_ge,
                                                   fill=k.fillreg(-1.0), base=qb, channel_multiplier=1), R=[imp], W=[imp])
            k.op("dve", lambda e: e.max(out=m8a[:], in_=sc), R=[imp], W=[m8a])
            k.op("dve", lambda e: e.match_replace(out=sc2[:], in_to_replace=m8a[:], in_values=sc, imm_value=-3e38),
                 R=[imp, m8a], W=[sc2])
            k.op("dve", lambda e: e.max(out=m8b[:], in_=sc2[:]), R=[sc2], W=[m8b])
            k.op("dve", lambda e: e.tensor_scalar(out=sc2[:], in0=sc, scalar1=m8b[:, 7:8], scalar2=1.0,
                                                  op0=ALU.is_ge, op1=ALU.subtract), R=[imp, m8b, sc2], W=[sc2])
            k.op("dve", lambda e: e.tensor_scalar(out=bsel[:, 0:NSLC], in0=sc2[:], scalar1=30000.0, scalar2=None,
                                                  op0=ALU.mult), R=[sc2], W=[bsel])
            pv = ptr[:].bitcast(BF16)
            for c in range(NJC):
                k.op("pe", lambda e: e.transpose(out=pv[:, c * 128:(c + 1) * 128], in_=bsel[:, c * 128:(c + 1) * 128],
                                                 identity=ident_b[:]), R=[bsel, ident_b], W=[ptr])
                k.op("act", lambda e: e.activation(out=biasT[:, c, qb:qb + 128], in_=pv[:, c * 128:(c + 1) * 128],
                                                   func=AF.Copy), R=[ptr], W=[biasT])
    k.pop_scope()
    k.push_scope()
    Ibig = k.sb([128, 8192], BF16, "Ibig")
    k.op("pool", lambda e: e.memset(Ibig[:], 1.0), W=[Ibig])
    k.op("pool", lambda e: e.affine_select(out=Ibig[:], in_=Ibig[:], pattern=[[1, 8192]], compare_op=ALU.is_ge,
                                           fill=k.fillreg(0.0), base=0, channel_multiplier=-64), R=[Ibig], W=[Ibig])
    k.op("pool", lambda e: e.affine_select(out=Ibig[:], in_=Ibig[:], pattern=[[-1, 8192]], compare_op=ALU.is_ge,
                                           fill=k.fillreg(0.0), base=63, channel_multiplier=64), R=[Ibig], W=[Ibig])
    KsKw = k.sb([128, S], BF16, "KsKw")
    k.dma("sp", KsKw[:], KsKw_d.t, R=[KsKw_d], W=[KsKw])
    V1s = k.sb([128, NKT, 65], BF16, "V1s")
    V1wr = [k.sb([128, 8, 65], BF16, "V1w%d" % i) for i in range(2)]
    ocr = [k.sb([128, 4, 64], BF16, "ocr%d" % i) for i in range(2)]
    k.dma("sp", V1s[:], V1s_d.t, R=[V1s_d], W=[V1s])
    Qd = k.sb([128, S], BF16, "Qd")
    ctx2 = AttnCtx(k, psum[0:4], n_pt=4)
    accS, accW = psum[4], psum[5]
    rs = k.sb([128, 1], F32, "rs")
    rw = k.sb([128, 1], F32, "rw")
    of = k.sb([128, 64], F32, "of")
    ob = [k.sb([128, 4, 64], BF16, "nob%d" % i) for i in range(2)]
    oi = 0
    for r in range(2):
        k.dma("sp", Qd[:], Qdup_d.t[r], R=[Qdup_d], W=[Qd])
        for qt in range(NQT):
            q0 = qt * NQ
            nk = (q0 + NQ) // 128
            accs = [(accS, accS[:, s * 65:(s + 1) * 65], s == 0) for s in range(NS)]
            for kt in range(nk):
                c = (kt * 2) // 128
                ktl = kt - c * 64
                attn_unit(ctx2, KsKw[0:64, kt * 128:(kt + 1) * 128], Qd[0:64, q0:q0 + NQ], [KsKw], [Qd], scale,
                          V1s[:, kt, :], [V1s], accs, first=(kt == 0), last=(kt == nk - 1),
                          masks=causal_masks(q0, kt * 128, NQ), nq=NQ,
                          bias_mm=(Ibig[0:JW, ktl * 128:(ktl + 1) * 128], biasT[0:JW, c, q0:q0 + NQ], [Ibig, biasT]))
            accs = [(accW, accW[:, s * 65:(s + 1) * 65], s == 0) for s in range(NS)]
            kts = [kt for kt in range(nk) if kt * 128 + 127 > q0 - 512]
            V1w = V1wr[oi % 2]
            oc = ocr[oi % 2]
            k.dma("sp", V1w[:, 0:len(kts), :], V1w_d.t[:, kts[0]:kts[0] + len(kts), :], R=[V1w_d], W=[V1w])
            k.dma("sp", oc[:, 0:NS, :], oc_d.t[q0:q0 + NQ, r * 64:(r + 1) * 64].rearrange("(s p) d -> p s d", p=128),
                  R=[oc_d], W=[oc])
            for i, kt in enumerate(kts):
                k0 = kt * 128
                masks = causal_masks(q0, k0, NQ)
                if k0 < q0:
                    masks = masks + [dict(base=k0 - q0 + 511, cm=1, step=-1)]
                attn_unit(ctx2, KsKw[64:128, k0:k0 + 128], Qd[64:128, q0:q0 + NQ], [KsKw], [Qd], scale,
                          V1w[:, i, :], [V1w], accs, first=(i == 0), last=(i == len(kts) - 1), masks=masks, nq=NQ)
            o = ob[oi % 2]
            oi += 1
            for s in range(NS):
                qb = qt * NS + s
                aS = accS[:, s * 65:(s + 1) * 65]
                aW = accW[:, s * 65:(s + 1) * 65]
                k.op("dve", lambda e: e.reciprocal(out=rs[:], in_=aS[:, 64:65]), R=[accS], W=[rs])
                k.op("dve", lambda e: e.reciprocal(out=rw[:], in_=aW[:, 64:65]), R=[accW], W=[rw])
                k.op("dve", lambda e: e.tensor_tensor(out=rs[:], in0=rs[:], in1=gate[:, qb, r, 1:2], op=ALU.mult),
                     R=[rs, gate], W=[rs])
                k.op("dve", lambda e: e.tensor_tensor(out=rw[:], in0=rw[:], in1=gate[:, qb, r, 2:3], op=ALU.mult),
                     R=[rw, gate], W=[rw])
                k.op("dve", lambda e: e.tensor_scalar(out=of[:], in0=oc[:, s, :], scalar1=gate[:, qb, r, 0:1],
                                                      scalar2=None, op0=ALU.mult), R=[oc, gate], W=[of])
                k.op("dve", lambda e: e.scalar_tensor_tensor(out=of[:], in0=aS[:, 0:64], scalar=rs[:], in1=of[:],
                                                             op0=ALU.mult, op1=ALU.add), R=[accS, rs, of], W=[of])
                k.op("dve", lambda e: e.scalar_tensor_tensor(out=o[:, s, :], in0=aW[:, 0:64], scalar=rw[:], in1=of[:],
                                                             op0=ALU.mult, op1=ALU.add), R=[accW, rw, of], W=[o])
            k.dma("sp", O_d.t[q0:q0 + NQ, r * 64:(r + 1) * 64].rearrange("(s p) d -> p s d", p=128), o[:, 0:NS, :],
                  R=[o], W=[O_d])
    k.pop_scope()


import math
import ml_dtypes
from concourse.bass_utils import run_bass_kernel_spmd

NCORES = 8
BF = ml_dtypes.bfloat16


def _rope_tables(pos, dim):
    inv = (np.float32(10000.0) ** (-np.arange(0, dim, 2, dtype=np.float32) / np.float32(dim))).astype(np.float32)
    ang = pos.astype(np.float32)[:, None] * inv[None, :]
    return np.cos(ang).astype(np.float32), np.sin(ang).astype(np.float32)


def _launch(build, in_maps):
    k = KB()
    outs = build(k)
    k.finish(outs)
    k.close()
    res = run_bass_kernel_spmd(k.nc, in_maps, core_ids=list(range(len(in_maps))))
    return res.results


def _psum(k):
    return [k.ps([128, 512], F32, "pb%d" % i) for i in range(8)]


def _xT_shards(h, S):
    T = S // NCORES
    out = []
    for c in range(NCORES):
        out.append(np.ascontiguousarray(h[c * T:(c + 1) * T].T.reshape(8, 128, T).transpose(1, 0, 2)))
    return out


def _swap_cols(w, nheads, d):
    half = d // 2
    idx = np.concatenate([np.concatenate([np.arange(hh * d + half, hh * d + d), np.arange(hh * d, hh * d + half)])
                          for hh in range(nheads)])
    return np.ascontiguousarray(w[:, idx])


def _tables64(pos):
    cos, sin = _rope_tables(pos, 64)
    c2 = np.ascontiguousarray(np.tile(cos.T, (4, 1)))
    s2 = np.ascontiguousarray(np.tile(np.concatenate([-sin.T, sin.T], 0), (2, 1)))
    return c2, s2


def _OT_shards(O_all, S):
    T = S // NCORES
    return [np.ascontiguousarray(O_all[c * T:(c + 1) * T].T.reshape(8, 128, T).transpose(1, 0, 2)) for c in range(NCORES)]


def _v1_head_layout(v, S):
    return np.ascontiguousarray(v.reshape(S // 128, 128, v.shape[-1]).transpose(1, 0, 2))


def run_post(S, O_all, h, i, P):
    T = S // NCORES
    NT = T // 128
    d = {}

    def build(k):
        OTd = k.dram("OTd", [128, 8, T], BF16, "ExternalInput")
        h_d = k.dram("h", [T, 1024], F32, "ExternalInput")
        wout_d = k.dram("wout", [1024, 1024], F32, "ExternalInput")
        v = {n: k.dram(n, [1024], F32, "ExternalInput") for n in ("ln1g", "ln1b", "ln2g", "ln2b")}
        wr_d = k.dram("wr", [1024, 32], F32, "ExternalInput")
        br_d = k.dram("br", [32], F32, "ExternalInput")
        wgu_d = k.dram("wgu", [32, 1024, 2048], F32, "ExternalInput")
        bgu_d = k.dram("bgu", [32, 2048], F32, "ExternalInput")
        wd_d = k.dram("wd", [32, 1024, 1024], F32, "ExternalInput")
        bd_d = k.dram("bd", [32, 1024], F32, "ExternalInput")
        out_d = k.dram("out", [T, 1024], F32, "ExternalOutput")
        XT = k.sb([128, 8, T], BF16, "XT")
        k.dma("sp", XT[:], OTd.t, R=[OTd], W=[XT])
        emit_post(k, NT, XT, h_d, wout_d, v["ln1g"], v["ln1b"], v["ln2g"], v["ln2b"], wr_d, br_d, wgu_d, bgu_d,
                  wd_d, bd_d, out_d)
        return [out_d]

    OTs = _OT_shards(O_all, S)
    common = dict(wout=P["wout"], ln1g=P["ln1_g"][i], ln1b=P["ln1_b"][i], ln2g=P["ln2_g"][i], ln2b=P["ln2_b"][i],
                  wr=P["moe_w_router"][i], br=P["moe_b_router"][i], wgu=P["moe_w_gu"][i], bgu=P["moe_b_gu"][i],
                  wd=P["moe_w_down"][i], bd=P["moe_b_down"][i])
    ins = [dict(OTd=OTs[c], h=np.ascontiguousarray(h[c * T:(c + 1) * T]), **common) for c in range(NCORES)]
    res = _launch(build, ins)
    return np.concatenate([r["out"] for r in res], 0)


def run_da(S, h, i, j, P):
    T = S // NCORES
    lam_init = 0.8 - 0.6 * math.exp(-0.3 * i)

    def buildA(k):
        xT_d = k.dram("xT", [128, 8, T], F32, "ExternalInput")
        w_d = k.dram("w", [1024, 3072], F32, "ExternalInput")
        wsw_d = k.dram("wsw", [1024, 2048], F32, "ExternalInput")
        c2_d = k.dram("c2", [128, T], F32, "ExternalInput")
        s2_d = k.dram("s2", [128, T], F32, "ExternalInput")
        QT_d = k.dram("QT", [8, 128, T], BF16, "ExternalOutput")
        KT_d = k.dram("KT", [8, 128, T], BF16, "ExternalOutput")
        V1_d = k.dram("V1", [T, 8, 129], BF16, "ExternalOutput")
        emit_da_proj(k, T, xT_d, w_d, wsw_d, c2_d, s2_d, QT_d, KT_d, V1_d, _psum(k))
        return [QT_d, KT_d, V1_d]

    w = P["da_w_in"][j]
    wsw = _swap_cols(w[:, :2048], 32, 64)
    xTs = _xT_shards(h, S)
    ins = []
    for c in range(NCORES):
        c2, s2 = _tables64(np.arange(c * T, (c + 1) * T))
        ins.append(dict(xT=xTs[c], w=w, wsw=wsw, c2=c2, s2=s2))
    rA = _launch(buildA, ins)
    QT = np.concatenate([r["QT"] for r in rA], 2)
    KT = np.concatenate([r["KT"] for r in rA], 2)
    V1 = np.concatenate([r["V1"] for r in rA], 0)

    def buildB(k):
        QTh = k.dram("QTh", [128, S], BF16, "ExternalInput")
        KTh = k.dram("KTh", [128, S], BF16, "ExternalInput")
        V1h = k.dram("V1h", [128, S // 128, 129], BF16, "ExternalInput")
        ls = [k.dram(n, [64], F32, "ExternalInput") for n in ("lq1", "lk1", "lq2", "lk2")]
        g_d = k.dram("g", [128], F32, "ExternalInput")
        O_d = k.dram("O", [S, 128], BF16, "ExternalOutput")
        emit_da_attn(k, S, QTh, KTh, V1h, ls[0], ls[1], ls[2], ls[3], g_d, O_d, lam_init, _psum(k))
        return [O_d]

    ins = []
    for hd in range(8):
        ins.append(dict(QTh=np.ascontiguousarray(QT[hd]), KTh=np.ascontiguousarray(KT[hd]),
                        V1h=_v1_head_layout(V1[:, hd, :], S), lq1=P["da_lambda_q1"][j], lk1=P["da_lambda_k1"][j],
                        lq2=P["da_lambda_q2"][j], lk2=P["da_lambda_k2"][j], g=P["da_subln"][j]))
    rB = _launch(buildB, ins)
    O_all = np.concatenate([r["O"] for r in rB], 1)
    return run_post(S, O_all, h, i, dict(P, wout=P["da_w_out"][j]))


def run_mla(S, h, i, j, P):
    T = S // NCORES

    def buildA(k):
        xT_d = k.dram("xT", [128, 8, T], F32, "ExternalInput")
        win_d = k.dram("win", [1024, 416], F32, "ExternalInput")
        qn_d = k.dram("qn", [256], F32, "ExternalInput")
        kvn_d = k.dram("kvn", [128], F32, "ExternalInput")
        wuq_d = k.dram("wuq", [256, 1536], F32, "ExternalInput")
        wuqsw_d = k.dram("wuqsw", [256, 512], F32, "ExternalInput")
        wukv_d = k.dram("wukv", [128, 2048], F32, "ExternalInput")
        cs16_d = k.dram("cs16", [T, 2, 16], F32, "ExternalInput")
        c2_d = k.dram("c2", [128, T], F32, "ExternalInput")
        s2_d = k.dram("s2", [128, T], F32, "ExternalInput")
        QT_d = k.dram("QT", [16, 96, T], BF16, "ExternalOutput")
        KnT_d = k.dram("KnT", [16, 64, T], BF16, "ExternalOutput")
        KrT_d = k.dram("KrT", [32, T], BF16, "ExternalOutput")
        V1_d = k.dram("V1", [T, 16, 65], BF16, "ExternalOutput")
        emit_mla_proj(k, T, xT_d, win_d, qn_d, kvn_d, wuq_d, wuqsw_d, wukv_d, cs16_d, c2_d, s2_d,
                      QT_d, KnT_d, KrT_d, V1_d, _psum(k))
        return [QT_d, KnT_d, KrT_d, V1_d]

    wuq = P["mla_w_uq"][j]
    idx = np.concatenate([np.concatenate([np.arange(hh * 96 + 80, hh * 96 + 96), np.arange(hh * 96 + 64, hh * 96 + 80)])
                          for hh in range(16)])
    wuqsw = np.ascontiguousarray(wuq[:, idx])
    xTs = _xT_shards(h, S)
    ins = []
    for c in range(NCORES):
        cos, sin = _rope_tables(np.arange(c * T, (c + 1) * T), 32)
        c2 = np.zeros((128, T), np.float32)
        s2 = np.zeros((128, T), np.float32)
        c2[64:96] = np.concatenate([cos.T, cos.T], 0)
        s2[64:96] = np.concatenate([-sin.T, sin.T], 0)
        cs16 = np.ascontiguousarray(np.stack([cos, sin], 1))
        ins.append(dict(xT=xTs[c], win=P["mla_w_in"][j], qn=P["mla_q_norm"][j], kvn=P["mla_kv_norm"][j], wuq=wuq,
                        wuqsw=wuqsw, wukv=P["mla_w_ukv"][j], cs16=cs16, c2=c2, s2=s2))
    rA = _launch(buildA, ins)
    QT = np.concatenate([r["QT"] for r in rA], 2)
    KnT = np.concatenate([r["KnT"] for r in rA], 2)
    KrT = np.concatenate([r["KrT"] for r in rA], 1)
    V1 = np.concatenate([r["V1"] for r in rA], 0)

    def buildB(k):
        QT_d = k.dram("QTh", [2, 96, S], BF16, "ExternalInput")
        KT_d = k.dram("KTh", [2, 96, S], BF16, "ExternalInput")
        V1_d = k.dram("V1h", [2, 128, S // 128, 65], BF16, "ExternalInput")
        O_d = k.dram("O", [S, 128], BF16, "ExternalOutput")
        emit_mla_attn(k, S, QT_d, KT_d, V1_d, O_d, _psum(k))
        return [O_d]

    ins = []
    for c in range(NCORES):
        hs = (2 * c, 2 * c + 1)
        ins.append(dict(QTh=np.ascontiguousarray(QT[list(hs)]),
                        KTh=np.ascontiguousarray(np.stack([np.concatenate([KnT[hh], KrT], 0) for hh in hs], 0)),
                        V1h=np.stack([_v1_head_layout(V1[:, hh, :], S) for hh in hs], 0)))
    rB = _launch(buildB, ins)
    O_all = np.concatenate([r["O"] for r in rB], 1)
    return run_post(S, O_all, h, i, dict(P, wout=P["mla_w_out"][j]))


def run_nsa(S, h, i, j, P):
    T = S // NCORES
    NC = S // 16
    NB = NC // NCORES
    NSLC = S // 64
    W = 16 * NB + 16

    def buildA(k):
        xT_d = k.dram("xT", [128, 8, T], F32, "ExternalInput")
        w_d = k.dram("w", [1024, 2608], F32, "ExternalInput")
        wsw_d = k.dram("wsw", [1024, 1536], F32, "ExternalInput")
        c2_d = k.dram("c2", [128, T], F32, "ExternalInput")
        s2_d = k.dram("s2", [128, T], F32, "ExternalInput")
        QT_d = k.dram("QT", [8, 128, T], BF16, "ExternalOutput")
        o2 = {n: k.dram(n, [2, 128, T], BF16, "ExternalOutput") for n in ("KsT", "KwT", "KcT", "VcT")}
        V1s_d = k.dram("V1s", [T, 4, 65], BF16, "ExternalOutput")
        V1w_d = k.dram("V1w", [T, 4, 65], BF16, "ExternalOutput")
        gate_d = k.dram("gate", [T, 48], F32, "ExternalOutput")
        emit_nsa_proj(k, T, xT_d, w_d, wsw_d, c2_d, s2_d, QT_d, o2["KsT"], o2["KwT"], o2["KcT"], o2["VcT"],
                      V1s_d, V1w_d, gate_d, _psum(k))
        return [QT_d, V1s_d, V1w_d, gate_d] + list(o2.values())

    w = P["nsa_w_in"][j]
    wsw = np.concatenate([_swap_cols(w[:, 0:1024], 16, 64), _swap_cols(w[:, 1536:1792], 4, 64),
                          _swap_cols(w[:, 2048:2304], 4, 64)], 1)
    xTs = _xT_shards(h, S)
    ins = []
    for c in range(NCORES):
        c2, s2 = _tables64(np.arange(c * T, (c + 1) * T))
        ins.append(dict(xT=xTs[c], w=w, wsw=np.ascontiguousarray(wsw), c2=c2, s2=s2))
    rA = _launch(buildA, ins)
    cat = lambda n, ax: np.concatenate([r[n] for r in rA], ax)
    QT = cat("QT", 2)
    KsT = cat("KsT", 2).reshape(4, 64, S)
    KwT = cat("KwT", 2).reshape(4, 64, S)
    KcT = cat("KcT", 2).reshape(4, 64, S)
    VcT = cat("VcT", 2).reshape(4, 64, S)
    V1s = cat("V1s", 0)
    V1w = cat("V1w", 0)
    gate = cat("gate", 0)

    def buildA2(k):
        wk = k.dram("win_k", [4, 64, W], BF16, "ExternalInput")
        wv = k.dram("win_v", [4, 64, W], BF16, "ExternalInput")
        pk = k.dram("posk", [32, 64], F32, "ExternalInput")
        pv = k.dram("posv", [32, 64], F32, "ExternalInput")
        w1k = k.dram("w1k", [2048, 256], F32, "ExternalInput")
        w1v = k.dram("w1v", [2048, 256], F32, "ExternalInput")
        w2k = k.dram("w2k", [256, 64], F32, "ExternalInput")
        w2ksw = k.dram("w2ksw", [256, 64], F32, "ExternalInput")
        w2v = k.dram("w2v", [256, 64], F32, "ExternalInput")
        c2_d = k.dram("c2", [64, NB], F32, "ExternalInput")
        s2_d = k.dram("s2", [64, NB], F32, "ExternalInput")
        KcT_d = k.dram("KcTo", [4, 64, NB], BF16, "ExternalOutput")
        V1c_d = k.dram("V1co", [4, NB, 65], BF16, "ExternalOutput")
        emit_nsa_compress(k, NB, wk, wv, pk, pv, w1k, w1v, w2k, w2ksw, w2v, c2_d, s2_d, KcT_d, V1c_d, _psum(k))
        return [KcT_d, V1c_d]

    KcP = np.concatenate([KcT, np.zeros((4, 64, 64), KcT.dtype)], 2)
    VcP = np.concatenate([VcT, np.zeros((4, 64, 64), VcT.dtype)], 2)
    w2k = P["nsa_cmp_k_w2"][j]
    ins = []
    for c in range(NCORES):
        n0 = c * NB
        cos, sin = _rope_tables(np.arange(n0, n0 + NB) * 16 + 31, 64)
        c2 = np.ascontiguousarray(np.concatenate([cos.T, cos.T], 0))
        s2 = np.ascontiguousarray(np.concatenate([-sin.T, sin.T], 0))
        ins.append(dict(win_k=np.ascontiguousarray(KcP[:, :, 16 * n0:16 * n0 + W]),
                        win_v=np.ascontiguousarray(VcP[:, :, 16 * n0:16 * n0 + W]),
                        posk=P["nsa_cmp_pos_k"][j], posv=P["nsa_cmp_pos_v"][j], w1k=P["nsa_cmp_k_w1"][j],
                        w1v=P["nsa_cmp_v_w1"][j], w2k=w2k, w2ksw=_swap_cols(w2k, 1, 64), w2v=P["nsa_cmp_v_w2"][j],
                        c2=c2, s2=s2))
    rA2 = _launch(buildA2, ins)
    Kc = np.concatenate([r["KcTo"] for r in rA2], 2)
    V1c = np.concatenate([r["V1co"] for r in rA2], 1)
    A = np.zeros((NC, NSLC), np.float32)
    jj = np.arange(NSLC)
    for off, wt in ((-1, 1.0), (0, 2.0), (1, 2.0), (2, 2.0), (3, 1.0)):
        n = 4 * jj + off
        ok = (n >= 0) & (n < NC - 1)
        A[n[ok], jj[ok]] = wt
    A = A.astype(BF)
    NCp = (NC + 127) // 128 * 128
    CW = 65 + NSLC

    def buildB(k):
        QT4_d = k.dram("QT4", [2, 128, S], BF16, "ExternalInput")
        Qdup_d = k.dram("Qdup", [2, 128, S], BF16, "ExternalInput")
        KcT2_d = k.dram("KcT2", [128, NCp], BF16, "ExternalInput")
        V1cA_d = k.dram("V1cA", [128, NCp // 128, CW], BF16, "ExternalInput")
        KsKw_d = k.dram("KsKw", [128, S], BF16, "ExternalInput")
        V1s_d = k.dram("V1sh", [128, S // 128, 65], BF16, "ExternalInput")
        V1w_d = k.dram("V1wh", [128, S // 128, 65], BF16, "ExternalInput")
        gate_d = k.dram("gateh", [128, S // 128, 2, 3], F32, "ExternalInput")
        O_d = k.dram("O", [S, 128], BF16, "ExternalOutput")
        oc_d = k.dram("ocs", [S, 128], BF16, "ExternalOutput")
        emit_nsa_attn(k, S, QT4_d, Qdup_d, KcT2_d, V1cA_d, KsKw_d, V1s_d, V1w_d, gate_d, O_d, oc_d, _psum(k))
        return [O_d, oc_d]

    ins = []
    for c in range(NCORES):
        g, sub = c // 2, c % 2
        own = QT[2 * g + sub]
        oth = QT[2 * g + 1 - sub]
        Qdup = np.stack([np.concatenate([own[r * 64:(r + 1) * 64]] * 2, 0) for r in range(2)], 0)
        kc2 = np.zeros((128, NCp), BF)
        kc2[0:64, :NC] = Kc[g]
        kc2[64:128, :NC] = Kc[g]
        vca = np.zeros((NCp, CW), BF)
        vca[:NC, 0:65] = V1c[g]
        vca[:NC, 65:] = A
        gh = gate[:, (2 * c) * 3:(2 * c + 2) * 3].reshape(S // 128, 128, 2, 3).transpose(1, 0, 2, 3)
        ins.append(dict(QT4=np.ascontiguousarray(np.stack([own, oth], 0)), Qdup=np.ascontiguousarray(Qdup), KcT2=kc2,
                        V1cA=_v1_head_layout(vca, NCp), KsKw=np.ascontiguousarray(np.concatenate([KsT[g], KwT[g]], 0)),
                        V1sh=_v1_head_layout(V1s[:, g, :], S), V1wh=_v1_head_layout(V1w[:, g, :], S),
                        gateh=np.ascontiguousarray(gh)))
    rB = _launch(buildB, ins)
    O_all = np.concatenate([r["O"] for r in rB], 1)
    return run_post(S, O_all, h, i, dict(P, wout=P["nsa_w_out"][j]))


def forward(P, S=16384, depth=4):
    P = {n: np.asarray(v) for n, v in P.items()}
    h = np.ascontiguousarray(P["x"][0].astype(np.float32))
    for i in range(depth):
        m, j = i % 3, i // 3
        if m == 0:
            h = run_da(S, h, i, j, P)
        elif m == 1:
            h = run_mla(S, h, i, j, P)
        else:
            h = run_nsa(S, h, i, j, P)
    return h[None].astype(np.float32)


def kernel(**inputs):
    return forward(inputs)
```

```python
import contextlib
import numpy as np
import concourse.bass as bass
import concourse.mybir as mybir

F32 = mybir.dt.float32
BF16 = mybir.dt.bfloat16
I32 = mybir.dt.int32
AF = mybir.ActivationFunctionType
ALU = mybir.AluOpType
AX = mybir.AxisListType


class Buf:
    __slots__ = ("t", "name", "lw", "rd", "excl")

    def __init__(self, t, name, excl=False):
        self.t = t
        self.name = name
        self.excl = excl
        self.lw = None
        self.rd = []

    def __getitem__(self, idx):
        return self.t[idx]


class KB:
    ENG = ("pe", "act", "dve", "pool", "sp")

    def __init__(self, n_dma_sems=12):
        self.nc = bass.Bass("TRN2", target_bir_lowering=False)
        self.es = contextlib.ExitStack()
        nc = self.nc
        self.eng = {"pe": nc.tensor, "act": nc.scalar, "dve": nc.vector,
                    "pool": nc.gpsimd, "sp": nc.sync}
        self.sems = {}
        self.cnt = {}
        for e in self.ENG:
            self.sems[e] = self.es.enter_context(nc.semaphore("s_" + e))
            self.cnt[e] = 0
        self.waited = {e: {} for e in self.ENG}
        self.dsem = {}
        self.dsem_i = {}
        for q in ("sp", "pool", "act"):
            lst = []
            for i in range(n_dma_sems):
                key = "d_%s_%d" % (q, i)
                self.sems[key] = self.es.enter_context(nc.semaphore(key))
                self.cnt[key] = 0
                lst.append(key)
            self.dsem[q] = lst
            self.dsem_i[q] = 0
        self.nbuf = 0
        self.ninstr = 0

    def sb(self, shape, dtype, name=None):
        self.nbuf += 1
        name = ("sb%d_" % self.nbuf) + (name or "")
        t = self.es.enter_context(self.nc.sbuf_tensor(name, list(shape), dtype))
        return Buf(t, name)

    def ps(self, shape, dtype=F32, name=None):
        self.nbuf += 1
        name = ("ps%d_" % self.nbuf) + (name or "")
        t = self.es.enter_context(self.nc.psum_tensor(name, list(shape), dtype))
        return Buf(t, name, excl=True)

    def dram(self, name, shape, dtype, kind):
        t = self.nc.dram_tensor(name, list(shape), dtype, kind=kind)
        return Buf(t.ap(), name)

    def _wait(self, e, tok):
        if tok is None:
            return
        key, val = tok
        w = self.waited[e]
        if w.get(key, 0) >= val:
            return
        self.eng[e].wait_ge(self.sems[key], val)
        w[key] = val

    def _deps(self, e, R, W, same_engine_sync):
        toks = []
        for b in R:
            if b.lw is not None:
                toks.append(b.lw)
        for b in W:
            if b.lw is not None:
                toks.append(b.lw)
            toks.extend(b.rd)
        for tok in toks:
            if tok[0] == e and not same_engine_sync:
                continue
            self._wait(e, tok)

    def op(self, e, fn, R=(), W=(), sync_same=None):
        if sync_same is None:
            sync_same = (e != "pe")
        if e != "pe":
            xr = [b for b in R if b.excl and b not in W]
            if xr:
                W = list(W) + xr
        self._deps(e, R, W, sync_same)
        ins = fn(self.eng[e])
        self.cnt[e] += 1
        self.ninstr += 1
        tok = (e, self.cnt[e])
        ins.then_inc(self.sems[e], 1)
        for b in W:
            b.lw = tok
            b.rd = []
        for b in R:
            if b not in W:
                b.rd.append(tok)
                if len(b.rd) > 64:
                    b.rd = self._compact(b.rd)
        return tok

    @staticmethod
    def _compact(rd):
        best = {}
        for k, v in rd:
            if best.get(k, 0) < v:
                best[k] = v
        return list(best.items())

    def dma(self, q, out_ap, in_ap, R=(), W=(), **kw):
        self._deps(q, R, W, True)
        lst = self.dsem[q]
        key = lst[self.dsem_i[q] % len(lst)]
        self.dsem_i[q] += 1
        if self.cnt[key] > 0:
            self._wait(q, (key, self.cnt[key]))
        ins = self.eng[q].dma_start(out=out_ap, in_=in_ap, **kw)
        self.cnt[key] += 16
        self.ninstr += 1
        tok = (key, self.cnt[key])
        ins.then_inc(self.sems[key], 16)
        for b in W:
            b.lw = tok
            b.rd = []
        for b in R:
            if b not in W:
                b.rd.append(tok)
        return tok

    def fillreg(self, val):
        c = getattr(self, "_fregs", None)
        if c is None:
            c = self._fregs = {}
        if val not in c:
            c[val] = self.nc.gpsimd.to_reg(val)
        return c[val]

    def barrier(self):
        toks = [(e, self.cnt[e]) for e in self.ENG if self.cnt[e] > 0]
        for q in self.dsem:
            for key in self.dsem[q]:
                if self.cnt[key] > 0:
                    toks.append((key, self.cnt[key]))
        for e in self.ENG:
            for tok in toks:
                if tok[0] != e:
                    self._wait(e, tok)

    def push_scope(self):
        self._outer = getattr(self, "_outer", [])
        self._outer.append(self.es)
        self.es = contextlib.ExitStack()

    def pop_scope(self):
        self.barrier()
        self.es.close()
        self.es = self._outer.pop()

    def finish(self, bufs):
        for b in bufs:
            if b.lw is not None:
                self._wait("sp", b.lw)

    def close(self):
        self.es.close()


ALPHA = 8.0 ** 0.25
LN_EPS = 1e-5
D = 1024
NE = 32
FE = 1024


def make_ident(k, dtype, name):
    idt = k.sb([128, 128], dtype, name)
    k.op("pool", lambda e: e.memset(idt[:], 1.0), W=[idt])
    k.op("pool", lambda e: e.affine_select(out=idt[:], in_=idt[:], pattern=[[-1, 128]],
                                           compare_op=ALU.is_equal, fill=k.fillreg(0.0), base=0,
                                           channel_multiplier=1), R=[idt], W=[idt])
    return idt


def bcast_load(k, q, dst, src_ap_1d, n):
    pass


def layer_norm_block(k, Zb, zap, g_t, b_t, tmp):
    st, mv, rstd = tmp
    for c in range(2):
        k.op("dve", lambda e, c=c: e.bn_stats(out=st[:, c, :], in_=zap[:, c * 512:(c + 1) * 512]),
             R=[Zb], W=[st])
    k.op("dve", lambda e: e.bn_aggr(out=mv[:], in_=st[:].rearrange("p a b -> p (a b)")), R=[st], W=[mv])
    k.op("act", lambda e: e.activation(out=rstd[:], in_=mv[:, 1:2], func=AF.Sqrt, bias=eps_ap(k), scale=1.0),
         R=[mv], W=[rstd])
    k.op("dve", lambda e: e.reciprocal(out=rstd[:], in_=rstd[:]), R=[rstd], W=[rstd])
    k.op("dve", lambda e: e.tensor_scalar(out=zap, in0=zap, scalar1=mv[:, 0:1], scalar2=rstd[:, 0:1],
                                          op0=ALU.subtract, op1=ALU.mult), R=[Zb, mv, rstd], W=[Zb])
    k.op("pool", lambda e: e.tensor_tensor(out=zap, in0=zap, in1=g_t[:], op=ALU.mult), R=[Zb, g_t], W=[Zb])
    k.op("pool", lambda e: e.tensor_tensor(out=zap, in0=zap, in1=b_t[:], op=ALU.add), R=[Zb, b_t], W=[Zb])


_EPS = {}


def eps_ap(k):
    if getattr(k, "_eps_t", None) is None:
        t = k.sb([128, 1], F32, "eps_c")
        k.op("pool", lambda e: e.memset(t[:], LN_EPS), W=[t])
        k._eps_t = t
    return k._eps_t[:]


def emit_post(k, NT, OT, h_d, wout_d, ln1g_d, ln1b_d, ln2g_d, ln2b_d, wr_d, br_d,
              wgu_d, bgu_d, wd_d, bd_d, out_d, ne=NE, psum=None):
    T = NT * 128
    XT = OT
    OTb = [Buf(XT.t, "otb%d" % i) for i in range(NT)]
    if XT.lw is not None:
        for b in OTb:
            b.lw = XT.lw
    ident_f = make_ident(k, F32, "ident_f")
    lng = k.sb([128, D], F32, "lng")
    lnb = k.sb([128, D], F32, "lnb")
    k.dma("sp", lng[:], ln1g_d.t.partition_broadcast(128), R=[ln1g_d], W=[lng])
    k.dma("sp", lnb[:], ln1b_d.t.partition_broadcast(128), R=[ln1b_d], W=[lnb])
    wr = k.sb([128, 8, NE], F32, "wr")
    k.dma("sp", wr[:], wr_d.t.rearrange("(c p) e -> p c e", p=128), R=[wr_d], W=[wr])
    brb = k.sb([128, NE], F32, "brb")
    k.dma("sp", brb[:], br_d.t.partition_broadcast(128), R=[br_d], W=[brb])
    bd = k.sb([NE, D], F32, "bd")
    k.dma("sp", bd[:], bd_d.t, R=[bd_d], W=[bd])
    bguT = k.sb([128, 8, 2, NE], F32, "bguT")
    if psum is None:
        psum = [k.ps([128, 512], F32, "pb%d" % i) for i in range(8)]
    pg, pl, py, pm = psum[0:2], psum[2:4], psum[4:6], psum[6:8]
    import os
    STAGE = int(os.environ.get("STAGE", "9"))
    SUB = int(os.environ.get("SUB", "9"))
    NW = 10
    wgu_ring = [k.sb([128, 2 * FE], BF16, "wgu%d" % i) for i in range(NW)]
    wd_ring = [k.sb([128, D], BF16, "wd%d" % i) for i in range(NW)]
    ring_i = {"gu": 0, "d": 0}

    def load_gu(src_ap_fn):
        bufs = []
        for kc in range(8):
            b = wgu_ring[ring_i["gu"] % NW]
            ring_i["gu"] += 1
            bufs.append(b)
        return bufs

    wo = []
    for kc in range(8):
        b = wd_ring[ring_i["d"] % NW]
        ring_i["d"] += 1
        k.dma("pool", b[:], wout_d.t[kc * 128:(kc + 1) * 128, :], R=[wout_d], W=[b])
        wo.append(b)

    Z = [k.sb([128, D], F32, "Z%d" % i) for i in range(NT)]
    G = k.sb([128, NT, NE], F32, "G")
    st = k.sb([128, 2, 6], F32, "ln_st")
    mv = k.sb([128, 2], F32, "ln_mv")
    rstd = k.sb([128, 1], F32, "ln_rstd")
    lntmp = (st, mv, rstd)
    eps_ap(k)
    k.push_scope()
    bgu_raw = k.sb([NE, 2 * FE], F32, "bgu_raw")
    k.dma("sp", bgu_raw[:], bgu_d.t, R=[bgu_d], W=[bgu_raw])
    for c in range(8):
        for gl in range(2):
            pb = pm[(c * 2 + gl) % 2]
            k.op("pe", lambda e, c=c, gl=gl, pb=pb: e.transpose(
                out=pb[:, 0:NE], in_=bgu_raw[:, 2 * 128 * c + gl: 2 * 128 * (c + 1): 2],
                identity=ident_f[0:NE, 0:NE]), R=[bgu_raw, ident_f], W=[pb])
            k.op("dve", lambda e, c=c, gl=gl, pb=pb: e.tensor_copy(out=bguT[:, c, gl, :], in_=pb[:, 0:NE]),
                 R=[pb], W=[bguT])

    xT32 = [k.sb([128, 8, 128], F32, "xT32_%d" % i) for i in range(2)]
    L = k.sb([128, NE], F32, "L")
    M8 = k.sb([128, 8], F32, "M8")
    nm = k.sb([128, 1], F32, "nm")
    msk = k.sb([128, NE], F32, "msk")
    Ee = k.sb([128, NE], F32, "Ee")
    ssum = k.sb([128, 1], F32, "ssum")
    GT = [k.sb([NE, 128], F32, "GT%d" % i) for i in range(2)]

    for tb in range(NT if STAGE >= 2 else 0):
        Zb = Z[tb]
        k.dma("sp", Zb[:], h_d.t[tb * 128:(tb + 1) * 128, :], R=[h_d], W=[Zb])
        for half in range(2):
            pb = py[half]
            for kc in range(8):
                k.op("pe", lambda e, kc=kc, half=half, pb=pb: e.matmul(
                    pb[:], lhsT=XT.t[:, kc, tb * 128:(tb + 1) * 128], rhs=wo[kc][:, half * 512:(half + 1) * 512],
                    start=(kc == 0), stop=(kc == 7)), R=[OTb[tb], wo[kc]], W=[pb])
            k.op("dve", lambda e, half=half, pb=pb: e.scalar_tensor_tensor(
                out=Zb[:, half * 512:(half + 1) * 512], in0=Zb[:, half * 512:(half + 1) * 512],
                scalar=ALPHA, in1=pb[:], op0=ALU.mult, op1=ALU.add), R=[Zb, pb], W=[Zb])
        if SUB < 2:
            continue
        layer_norm_block(k, Zb, Zb[:], lng, lnb, lntmp)
        if SUB < 3:
            continue
        x32 = xT32[tb % 2]
        for c4 in range(2):
            pb = pm[c4]
            for j in range(4):
                c = c4 * 4 + j
                k.op("pe", lambda e, c=c, j=j, pb=pb: e.transpose(
                    out=pb[:, j * 128:(j + 1) * 128], in_=Zb[:, c * 128:(c + 1) * 128],
                    identity=ident_f[:]), R=[Zb, ident_f], W=[pb])
            k.op("act", lambda e, c4=c4, pb=pb: e.activation(
                out=XT.t[:, c4 * 4:(c4 + 1) * 4, tb * 128:(tb + 1) * 128],
                in_=pb[:].rearrange("p (j t) -> p j t", j=4),
                func=AF.Copy), R=[pb], W=[OTb[tb]])
            k.op("dve", lambda e, c4=c4, pb=pb: e.tensor_copy(
                out=x32[:, c4 * 4:(c4 + 1) * 4, :], in_=pb[:].rearrange("p (j t) -> p j t", j=4)),
                R=[pb], W=[x32])
        if SUB < 4:
            continue
        pb = pm[0]
        for c in range(8):
            k.op("pe", lambda e, c=c, pb=pb: e.matmul(pb[:, 0:NE], lhsT=x32[:, c, :], rhs=wr[:, c, :],
                                                      start=(c == 0), stop=(c == 7)),
                 R=[x32, wr], W=[pb])
        k.op("dve", lambda e, pb=pb: e.tensor_tensor(out=L[:], in0=pb[:, 0:NE], in1=brb[:], op=ALU.add),
             R=[pb, brb], W=[L])
        k.op("dve", lambda e: e.max(out=M8[:], in_=L[:]), R=[L], W=[M8])
        k.op("dve", lambda e: e.tensor_scalar(out=msk[:], in0=L[:], scalar1=M8[:, 3:4], scalar2=None,
                                              op0=ALU.is_ge), R=[L, M8], W=[msk])
        k.op("dve", lambda e: e.tensor_scalar(out=nm[:], in0=M8[:, 0:1], scalar1=-1.0, scalar2=None,
                                              op0=ALU.mult), R=[M8], W=[nm])
        k.op("act", lambda e: e.activation(out=Ee[:], in_=L[:], func=AF.Exp, bias=nm[:], scale=1.0),
             R=[L, nm], W=[Ee])
        k.op("dve", lambda e: e.tensor_tensor(out=Ee[:], in0=Ee[:], in1=msk[:], op=ALU.mult),
             R=[Ee, msk], W=[Ee])
        k.op("dve", lambda e: e.reduce_sum(out=ssum[:], in_=Ee[:], axis=AX.X), R=[Ee], W=[ssum])
        k.op("dve", lambda e: e.reciprocal(out=ssum[:], in_=ssum[:]), R=[ssum], W=[ssum])
        k.op("dve", lambda e: e.tensor_scalar(out=G[:, tb, :], in0=Ee[:], scalar1=ssum[:, 0:1], scalar2=None,
                                              op0=ALU.mult), R=[Ee, ssum], W=[G])
        if SUB < 5:
            continue
        gt = GT[tb % 2]
        pb = pm[1]
        k.op("pe", lambda e, pb=pb: e.transpose(out=pb[0:NE, 0:128], in_=G[:, tb, :], identity=ident_f[:]),
             R=[G, ident_f], W=[pb])
        k.op("act", lambda e, pb=pb: e.activation(out=gt[:], in_=pb[0:NE, 0:128], func=AF.Copy),
             R=[pb], W=[gt])
        for half in range(2):
            pb = py[half]
            k.op("pe", lambda e, half=half, pb=pb: e.matmul(
                pb[:], lhsT=gt[:], rhs=bd[:, half * 512:(half + 1) * 512], start=True, stop=True),
                R=[gt, bd], W=[pb])
            k.op("dve", lambda e, half=half, pb=pb: e.scalar_tensor_tensor(
                out=Zb[:, half * 512:(half + 1) * 512], in0=Zb[:, half * 512:(half + 1) * 512],
                scalar=ALPHA, in1=pb[:], op0=ALU.mult, op1=ALU.add), R=[Zb, pb], W=[Zb])

    k.pop_scope()
    XT.lw = OTb[NT - 1].lw
    XT.rd = []
    k.dma("sp", lng[:], ln2g_d.t.partition_broadcast(128), R=[ln2g_d], W=[lng])
    k.dma("sp", lnb[:], ln2b_d.t.partition_broadcast(128), R=[ln2b_d], W=[lnb])

    TT = 512 if T >= 512 else T
    NTT = T // TT
    NSUB = TT // 128
    hg = [k.sb([128, TT], F32, "hg%d" % i) for i in range(2)]
    sg = [k.sb([128, TT], F32, "sg%d" % i) for i in range(2)]
    hl = [k.sb([128, TT], F32, "hl%d" % i) for i in range(2)]
    actT = [k.sb([128, 8, TT], BF16, "actT%d" % i) for i in range(2)]
    ui = 0
    ai = 0
    for ex in range(ne if STAGE >= 4 else 0):
        gu = []
        for kc in range(8):
            b = wgu_ring[ring_i["gu"] % NW]
            ring_i["gu"] += 1
            k.dma("pool", b[:], wgu_d.t[ex, kc * 128:(kc + 1) * 128, :], R=[wgu_d], W=[b])
            gu.append(b)
        dn = []
        for kc in range(8):
            b = wd_ring[ring_i["d"] % NW]
            ring_i["d"] += 1
            k.dma("pool", b[:], wd_d.t[ex, kc * 128:(kc + 1) * 128, :], R=[wd_d], W=[b])
            dn.append(b)
        for tt in range(NTT):
            at = actT[ai % 2]
            ai += 1
            for c in range(8):
                pgb = pg[ui % 2]
                plb = pl[ui % 2]
                hgb, sgb, hlb = hg[ui % 2], sg[ui % 2], hl[ui % 2]
                ui += 1
                for gl, pb in ((0, pgb), (1, plb)):
                    for kc in range(8):
                        k.op("pe", lambda e, kc=kc, gl=gl, pb=pb, c=c: e.matmul(
                            pb[:, 0:TT],
                            lhsT=gu[kc][:, 2 * 128 * c + gl: 2 * 128 * (c + 1): 2],
                            rhs=XT.t[:, kc, tt * TT:(tt + 1) * TT], start=(kc == 0), stop=(kc == 7)),
                            R=[gu[kc], XT], W=[pb])
                k.op("dve", lambda e, c=c, pgb=pgb, hgb=hgb: e.tensor_scalar(
                    out=hgb[:], in0=pgb[:, 0:TT], scalar1=bguT[:, c, 0, ex:ex + 1], scalar2=7.0,
                    op0=ALU.add, op1=ALU.min), R=[pgb, bguT], W=[hgb])
                k.op("act", lambda e, hgb=hgb, sgb=sgb: e.activation(
                    out=sgb[:], in_=hgb[:], func=AF.Sigmoid, scale=1.702), R=[hgb], W=[sgb])
                k.op("act", lambda e, c=c, plb=plb, hlb=hlb: e.activation(
                    out=hlb[:], in_=plb[:, 0:TT], func=AF.Identity, bias=bguT[:, c, 1, ex:ex + 1], scale=1.0),
                    R=[plb, bguT], W=[hlb])
                k.op("dve", lambda e, hlb=hlb: e.tensor_scalar(
                    out=hlb[:], in0=hlb[:], scalar1=7.0, scalar2=-7.0, op0=ALU.min, op1=ALU.max),
                    R=[hlb], W=[hlb])
                k.op("pool", lambda e, hgb=hgb, sgb=sgb: e.tensor_tensor(
                    out=sgb[:], in0=hgb[:], in1=sgb[:], op=ALU.mult), R=[hgb, sgb], W=[sgb])
                k.op("dve", lambda e, c=c, hlb=hlb, sgb=sgb, at=at: e.scalar_tensor_tensor(
                    out=at[:, c, :], in0=hlb[:], scalar=1.0, in1=sgb[:], op0=ALU.add, op1=ALU.mult),
                    R=[hlb, sgb], W=[at])
            for s in range(NSUB):
                tb = (tt * TT) // 128 + s
                for half in range(2):
                    pb = py[(s * 2 + half) % 2]
                    for c in range(8):
                        k.op("pe", lambda e, c=c, s=s, half=half, pb=pb: e.matmul(
                            pb[:], lhsT=at[:, c, s * 128:(s + 1) * 128],
                            rhs=dn[c][:, half * 512:(half + 1) * 512],
                            start=(c == 0), stop=(c == 7)), R=[at, dn[c]], W=[pb])
                    k.op("dve", lambda e, tb=tb, half=half, pb=pb: e.scalar_tensor_tensor(
                        out=Z[tb][:, half * 512:(half + 1) * 512], in0=pb[:], scalar=G[:, tb, ex:ex + 1],
                        in1=Z[tb][:, half * 512:(half + 1) * 512], op0=ALU.mult, op1=ALU.add),
                        R=[pb, G, Z[tb]], W=[Z[tb]])

    for tb in range(NT):
        if STAGE < 2:
            k.dma("sp", Z[tb][:], h_d.t[tb * 128:(tb + 1) * 128, :], R=[h_d], W=[Z[tb]])
        if STAGE >= 5:
            layer_norm_block(k, Z[tb], Z[tb][:], lng, lnb, lntmp)
        k.dma("sp", out_d.t[tb * 128:(tb + 1) * 128, :], Z[tb][:], R=[Z[tb]], W=[out_d])
    return Z


NEG_BIG = -30000.0


class AttnCtx:
    def __init__(self, k, sc_banks, n_pt=4):
        self.k = k
        self.sc = sc_banks
        self.pt = [k.sb([128, 512], BF16, "pt%d" % i) for i in range(n_pt)]
        self.ui = 0


def attn_unit(ctx, kt_ap, q_ap, Rk, Rq, scale, v_ap, Rv, accs, first, last, masks=(), nq=512,
              bias_mm=None, kparts=128):
    k = ctx.k
    sc = ctx.sc[ctx.ui % len(ctx.sc)]
    pt = ctx.pt[ctx.ui % len(ctx.pt)]
    ctx.ui += 1
    k.op("pe", lambda e: e.matmul(sc[0:kparts, 0:nq], lhsT=kt_ap, rhs=q_ap, start=True,
                                  stop=(bias_mm is None)), R=list(Rk) + list(Rq), W=[sc])
    if bias_mm is not None:
        ind_ap, b_ap, Rb = bias_mm
        k.op("pe", lambda e: e.matmul(sc[0:kparts, 0:nq], lhsT=ind_ap, rhs=b_ap, start=False, stop=True),
             R=list(Rb), W=[sc])
    k.op("act", lambda e: e.activation(out=pt[0:kparts, 0:nq], in_=sc[0:kparts, 0:nq], func=AF.Exp, scale=scale),
         R=[sc], W=[pt])
    for m in masks:
        k.op("pool", lambda e: e.affine_select(out=pt[0:kparts, 0:nq], in_=pt[0:kparts, 0:nq],
                                               pattern=[[m["step"], nq]], compare_op=ALU.is_ge, fill=k.fillreg(0.0),
                                               base=m["base"], channel_multiplier=m["cm"]),
             R=[pt], W=[pt])
    for s, (ab, aap, bank_first) in enumerate(accs):
        k.op("pe", lambda e: e.matmul(aap, lhsT=pt[0:kparts, s * 128:(s + 1) * 128], rhs=v_ap,
                                      start=(first and bank_first), stop=last, skip_group_check=True),
             R=[pt] + list(Rv), W=[ab])


def causal_masks(q0, k0, nq=512, kparts=128):
    if k0 + kparts - 1 <= q0:
        return []
    return [dict(base=q0 - k0, cm=-1, step=1)]


def run_units(ctx, units, depth=2):
    k = ctx.k
    pend = []
    n = len(units)
    for i in range(n + depth):
        if i < n:
            u = units[i]
            if u.get("pre") is not None:
                u["pre"]()
            nq = u.get("nq", 512)
            kparts = u.get("kparts", 128)
            sc = ctx.sc[ctx.ui % len(ctx.sc)]
            pt = ctx.pt[ctx.ui % len(ctx.pt)]
            ctx.ui += 1
            bias_mm = u.get("bias_mm")
            k.op("pe", lambda e: e.matmul(sc[0:kparts, 0:nq], lhsT=u["kt_ap"], rhs=u["q_ap"], start=True,
                                          stop=(bias_mm is None)), R=list(u["Rk"]) + list(u["Rq"]), W=[sc])
            if bias_mm is not None:
                ind_ap, b_ap, Rb = bias_mm
                k.op("pe", lambda e: e.matmul(sc[0:kparts, 0:nq], lhsT=ind_ap, rhs=b_ap, start=False, stop=True),
                     R=list(Rb), W=[sc])
            k.op("act", lambda e: e.activation(out=pt[0:kparts, 0:nq], in_=sc[0:kparts, 0:nq], func=AF.Exp,
                                               scale=u["scale"]), R=[sc], W=[pt])
            for m in u.get("masks", ()):
                k.op("pool", lambda e: e.affine_select(out=pt[0:kparts, 0:nq], in_=pt[0:kparts, 0:nq],
                                                       pattern=[[m["step"], nq]], compare_op=ALU.is_ge,
                                                       fill=k.fillreg(0.0), base=m["base"], channel_multiplier=m["cm"]),
                     R=[pt], W=[pt])
            pend.append((u, pt, kparts))
        j = i - depth
        if j >= 0:
            u, pt, kparts = pend[j]
            pend[j] = None
            for s, (ab, aap, bank_first) in enumerate(u["accs"]):
                k.op("pe", lambda e: e.matmul(aap, lhsT=pt[0:kparts, s * 128:(s + 1) * 128], rhs=u["v_ap"],
                                              start=(u["first"] and bank_first), stop=u["last"], skip_group_check=True),
                     R=[pt] + list(u["Rv"]), W=[ab])
            if u.get("after") is not None:
                u["after"]()


def emit_da_proj(k, T, xT_d, w_d, wsw_d, c2_d, s2_d, QT_d, KT_d, V1_d, psum):
    xT = k.sb([128, 8, T], BF16, "xT")
    k.dma("pool", xT[:], xT_d.t, R=[xT_d], W=[xT])
    w = k.sb([128, 8, 3072], BF16, "w_in")
    wsw = k.sb([128, 8, 2048], BF16, "w_sw")
    for kc in range(8):
        k.dma("pool", w[:, kc, :], w_d.t[kc * 128:(kc + 1) * 128, :], R=[w_d], W=[w])
        k.dma("pool", wsw[:, kc, :], wsw_d.t[kc * 128:(kc + 1) * 128, :], R=[wsw_d], W=[wsw])
    c2 = k.sb([128, T], F32, "c2")
    s2 = k.sb([128, T], F32, "s2")
    k.dma("sp", c2[:], c2_d.t, R=[c2_d], W=[c2])
    k.dma("sp", s2[:], s2_d.t, R=[s2_d], W=[s2])
    TT = min(512, T)
    t1 = [k.sb([128, TT], F32, "rt1_%d" % i) for i in range(2)]
    t2 = [k.sb([128, TT], F32, "rt2_%d" % i) for i in range(2)]
    ob = [k.sb([128, TT], BF16, "rob_%d" % i) for i in range(2)]
    ui = 0
    for which, dst in ((0, QT_d), (1, KT_d)):
        for h in range(8):
            col = which * 1024 + h * 128
            for tt in range(T // TT):
                pa = psum[(ui * 2) % 4]
                pb = psum[(ui * 2 + 1) % 4]
                a1, a2, o = t1[ui % 2], t2[ui % 2], ob[ui % 2]
                ui += 1
                for kc in range(8):
                    k.op("pe", lambda e: e.matmul(pa[:, 0:TT], lhsT=w[:, kc, col:col + 128],
                                                  rhs=xT[:, kc, tt * TT:(tt + 1) * TT], start=(kc == 0), stop=(kc == 7)),
                         R=[w, xT], W=[pa])
                for kc in range(8):
                    k.op("pe", lambda e: e.matmul(pb[:, 0:TT], lhsT=wsw[:, kc, col:col + 128],
                                                  rhs=xT[:, kc, tt * TT:(tt + 1) * TT], start=(kc == 0), stop=(kc == 7)),
                         R=[wsw, xT], W=[pb])
                k.op("dve", lambda e: e.tensor_tensor(out=a1[:], in0=pa[:, 0:TT], in1=c2[:, tt * TT:(tt + 1) * TT],
                                                      op=ALU.mult), R=[pa, c2], W=[a1])
                k.op("dve", lambda e: e.tensor_tensor(out=a2[:], in0=pb[:, 0:TT], in1=s2[:, tt * TT:(tt + 1) * TT],
                                                      op=ALU.mult), R=[pb, s2], W=[a2])
                k.op("pool", lambda e: e.tensor_tensor(out=o[:], in0=a1[:], in1=a2[:], op=ALU.add),
                     R=[a1, a2], W=[o])
                k.dma("sp", dst.t[h, :, tt * TT:(tt + 1) * TT], o[:], R=[o], W=[dst])
    vb = [k.sb([128, 8, 129], BF16, "vb%d" % i) for i in range(2)]
    for b in vb:
        k.op("pool", lambda e: e.memset(b[:], 1.0), W=[b])
    for tb in range(T // 128):
        v = vb[tb % 2]
        for half in range(2):
            pa = psum[4 + half]
            for kc in range(8):
                k.op("pe", lambda e: e.matmul(pa[:], lhsT=xT[:, kc, tb * 128:(tb + 1) * 128],
                                              rhs=w[:, kc, 2048 + half * 512:2048 + (half + 1) * 512],
                                              start=(kc == 0), stop=(kc == 7)), R=[xT, w], W=[pa])
            k.op("act", lambda e: e.activation(out=v[:, half * 4:(half + 1) * 4, 0:128],
                                               in_=pa[:].rearrange("p (h d) -> p h d", h=4), func=AF.Copy),
                 R=[pa], W=[v])
        k.dma("sp", V1_d.t[tb * 128:(tb + 1) * 128, :, :], v[:], R=[v], W=[V1_d])


def emit_da_attn(k, S, QT_d, KT_d, V1_d, lq1_d, lk1_d, lq2_d, lk2_d, g_d, O_d, lam_init, psum):
    NKT = S // 128
    NQ = min(512, S)
    NQT = S // NQ
    NS = NQ // 128
    KT = k.sb([128, S], BF16, "KT")
    V1 = k.sb([128, NKT, 129], BF16, "V1")
    nchunk = 4 if S >= 2048 else 1
    cs = S // nchunk
    for i in range(nchunk):
        k.dma("sp", KT[:, i * cs:(i + 1) * cs], KT_d.t[:, i * cs:(i + 1) * cs], R=[KT_d], W=[KT])
        k.dma("sp", V1[:, i * (NKT // nchunk):(i + 1) * (NKT // nchunk), :],
              V1_d.t[:, i * (NKT // nchunk):(i + 1) * (NKT // nchunk), :], R=[V1_d], W=[V1])
    lam4 = k.sb([128, 4, 64], F32, "lam4")
    for i, d in enumerate((lq1_d, lk1_d, lq2_d, lk2_d)):
        k.dma("sp", lam4[:, i, :], d.t.partition_broadcast(128), R=[d], W=[lam4])
    lprod = k.sb([128, 2, 64], F32, "lprod")
    lsum = k.sb([128, 2], F32, "lsum")
    nlam = k.sb([128, 1], F32, "nlam")
    k.op("dve", lambda e: e.tensor_tensor(out=lprod[:, 0, :], in0=lam4[:, 0, :], in1=lam4[:, 1, :], op=ALU.mult),
         R=[lam4], W=[lprod])
    k.op("dve", lambda e: e.tensor_tensor(out=lprod[:, 1, :], in0=lam4[:, 2, :], in1=lam4[:, 3, :], op=ALU.mult),
         R=[lam4], W=[lprod])
    k.op("dve", lambda e: e.reduce_sum(out=lsum[:], in_=lprod[:], axis=AX.X), R=[lprod], W=[lsum])
    k.op("act", lambda e: e.activation(out=lsum[:], in_=lsum[:], func=AF.Exp), R=[lsum], W=[lsum])
    k.op("dve", lambda e: e.tensor_tensor(out=nlam[:], in0=lsum[:, 1:2], in1=lsum[:, 0:1], op=ALU.subtract),
         R=[lsum], W=[nlam])
    k.op("dve", lambda e: e.tensor_scalar(out=nlam[:], in0=nlam[:], scalar1=-lam_init, scalar2=None, op0=ALU.add),
         R=[nlam], W=[nlam])
    gsc = k.sb([128, 128], F32, "gsc")
    k.dma("sp", gsc[:], g_d.t.partition_broadcast(128), R=[g_d], W=[gsc])
    k.op("dve", lambda e: e.tensor_scalar(out=gsc[:], in0=gsc[:], scalar1=(1.0 - lam_init), scalar2=None,
                                          op0=ALU.mult), R=[gsc], W=[gsc])
    eps = k.sb([128, 1], F32, "eps_da")
    k.op("pool", lambda e: e.memset(eps[:], 1e-5), W=[eps])

    ctx = AttnCtx(k, psum[0:4])
    def acc_of(g, s):
        bA, bB = psum[4 + 2 * (g % 2)], psum[5 + 2 * (g % 2)]
        if s < 3:
            return (bA, bA[:, s * 129:(s + 1) * 129], s == 0)
        return (bB, bB[:, 0:129], True)

    r1 = k.sb([128, 1], F32, "r1")
    r2 = k.sb([128, 1], F32, "r2")
    o1r = [k.sb([128, 4, 128], F32, "o1_%d" % i) for i in range(2)]
    oo = k.sb([128, 128], F32, "oo")
    sq = k.sb([128, 128], F32, "sq")
    ss = k.sb([128, 1], F32, "ss")
    ob = [k.sb([128, 4, 128], BF16, "ob%d" % i) for i in range(2)]
    scale = 64.0 ** -0.5
    qz = [[k.sb([128, NQ], BF16, "qz%d_%d" % (hh, i)) for i in range(2)] for hh in range(2)]
    for hh in range(2):
        for i in range(2):
            k.op("pool", lambda e: e.memset(qz[hh][i][:], 0.0), W=[qz[hh][i]])

    def make_after(qt, half, g):
        q0 = qt * NQ
        o1 = o1r[qt % 2]
        o = ob[qt % 2]

        def after():
            for s in range(NS):
                b, a, _ = acc_of(g, s)
                if half == 0:
                    k.op("dve", lambda e: e.reciprocal(out=r1[:], in_=a[:, 128:129]), R=[b], W=[r1])
                    k.op("dve", lambda e: e.tensor_scalar(out=o1[:, s, :], in0=a[:, 0:128], scalar1=r1[:], scalar2=None,
                                                          op0=ALU.mult), R=[b, r1], W=[o1])
                else:
                    k.op("dve", lambda e: e.reciprocal(out=r2[:], in_=a[:, 128:129]), R=[b], W=[r2])
                    k.op("dve", lambda e: e.tensor_tensor(out=r2[:], in0=r2[:], in1=nlam[:], op=ALU.mult),
                         R=[r2, nlam], W=[r2])
                    k.op("dve", lambda e: e.scalar_tensor_tensor(out=oo[:], in0=a[:, 0:128], scalar=r2[:],
                                                                 in1=o1[:, s, :], op0=ALU.mult, op1=ALU.add),
                         R=[b, r2, o1], W=[oo])
                    k.op("dve", lambda e: e.tensor_tensor(out=sq[:], in0=oo[:], in1=oo[:], op=ALU.mult),
                         R=[oo], W=[sq])
                    k.op("dve", lambda e: e.reduce_sum(out=ss[:], in_=sq[:], axis=AX.X), R=[sq], W=[ss])
                    k.op("dve", lambda e: e.tensor_scalar(out=ss[:], in0=ss[:], scalar1=1.0 / 128.0, scalar2=1e-5,
                                                          op0=ALU.mult, op1=ALU.add), R=[ss], W=[ss])
                    k.op("pool", lambda e: e.tensor_tensor(out=ss[:], in0=ss[:], in1=half_c[:], op=ALU.pow),
                         R=[ss, half_c], W=[ss])
                    k.op("dve", lambda e: e.scalar_tensor_tensor(out=o[:, s, :], in0=oo[:], scalar=ss[:], in1=gsc[:],
                                                                 op0=ALU.mult, op1=ALU.mult), R=[oo, ss, gsc], W=[o])
            if half == 1:
                k.dma("sp", O_d.t[q0:q0 + NQ, :].rearrange("(s p) d -> p s d", p=128), o[:, 0:NS, :],
                      R=[o], W=[O_d])
        return after

    half_c = k.sb([128, 1], F32, "half_c")
    k.op("pool", lambda e: e.memset(half_c[:], -0.5), W=[half_c])
    units = []
    g = 0
    for qt in range(NQT):
        q0 = qt * NQ
        nk = (q0 + NQ) // 128
        for half in range(2):
            accs = [acc_of(g, s) for s in range(NS)]
            pr = slice(half * 64, (half + 1) * 64)
            qb = qz[half][qt % 2]

            def pre(qb=qb, pr=pr, q0=q0):
                k.dma("sp", qb[pr, :], QT_d.t[pr, q0:q0 + NQ], R=[QT_d], W=[qb])
            for kt in range(nk):
                units.append(dict(kt_ap=KT[:, kt * 128:(kt + 1) * 128], q_ap=qb[:, :], Rk=[KT], Rq=[qb], scale=scale,
                                  v_ap=V1[:, kt, :], Rv=[V1], accs=accs, first=(kt == 0), last=(kt == nk - 1),
                                  masks=causal_masks(q0, kt * 128, NQ), nq=NQ, pre=(pre if kt == 0 else None),
                                  after=(make_after(qt, half, g) if kt == nk - 1 else None)))
            g += 1
    run_units(ctx, units)


def load_w_bf(k, w_d, K, N, name, col0=0):
    if K >= 128:
        KC = K // 128
        w = k.sb([128, KC, N], BF16, name)
        for kc in range(KC):
            k.dma("pool", w[:, kc, :], w_d.t[kc * 128:(kc + 1) * 128, col0:col0 + N], R=[w_d], W=[w])
    else:
        w = k.sb([K, 1, N], BF16, name)
        k.dma("pool", w[:, 0, :], w_d.t[:, col0:col0 + N], R=[w_d], W=[w])
    return w


class ProjCtx:
    def __init__(self, k, psum, TT):
        self.k = k
        self.psum = psum
        self.TT = TT
        self.t1 = [k.sb([128, TT], F32, "pj1_%d" % i) for i in range(2)]
        self.t2 = [k.sb([128, TT], F32, "pj2_%d" % i) for i in range(2)]
        self.ob = [k.sb([128, TT], BF16, "pjo_%d" % i) for i in range(3)]
        self.ui = 0


def proj_fm(pc, xT, KC, T, w, col, ncols, dst, dst_fn, rope=None, prow=0):
    k = pc.k
    TT = pc.TT
    pr = slice(prow, prow + ncols)
    for tt in range(T // TT):
        ts = slice(tt * TT, (tt + 1) * TT)
        pa = pc.psum[(pc.ui * 2) % 4]
        pb = pc.psum[(pc.ui * 2 + 1) % 4]
        a1, a2, o = pc.t1[pc.ui % 2], pc.t2[pc.ui % 2], pc.ob[pc.ui % 3]
        pc.ui += 1
        for kc in range(KC):
            k.op("pe", lambda e: e.matmul(pa[pr, 0:TT], lhsT=w[:, kc, col:col + ncols], rhs=xT[:, kc, ts],
                                          start=(kc == 0), stop=(kc == KC - 1)), R=[w, xT], W=[pa])
        if rope is None:
            k.op("act", lambda e: e.activation(out=o[pr, :], in_=pa[pr, 0:TT], func=AF.Copy), R=[pa], W=[o])
        else:
            wsw, col_sw, c2, s2 = rope
            for kc in range(KC):
                k.op("pe", lambda e: e.matmul(pb[pr, 0:TT], lhsT=wsw[:, kc, col_sw:col_sw + ncols], rhs=xT[:, kc, ts],
                                              start=(kc == 0), stop=(kc == KC - 1)), R=[wsw, xT], W=[pb])
            k.op("dve", lambda e: e.tensor_tensor(out=a1[pr, :], in0=pa[pr, 0:TT], in1=c2[pr, ts], op=ALU.mult),
                 R=[pa, c2], W=[a1])
            k.op("dve", lambda e: e.tensor_tensor(out=a2[pr, :], in0=pb[pr, 0:TT], in1=s2[pr, ts], op=ALU.mult),
                 R=[pb, s2], W=[a2])
            k.op("pool", lambda e: e.tensor_tensor(out=o[pr, :], in0=a1[pr, :], in1=a2[pr, :], op=ALU.add),
                 R=[a1, a2], W=[o])
        k.dma("sp", dst_fn(ts), o[pr, :], R=[o], W=[dst])


def proj_tm_v1(k, psum, xT, KC, T, w, wview_fn, nh, dv, dst):
    vb = [k.sb([128, nh, dv + 1], BF16, "v1b%d" % i) for i in range(2)]
    for b in vb:
        k.op("pool", lambda e: e.memset(b[:], 1.0), W=[b])
    for tb in range(T // 128):
        v = vb[tb % 2]
        pa = psum[4 + tb % 2]
        for kc in range(KC):
            k.op("pe", lambda e: e.matmul(pa[:, 0:nh * dv], lhsT=xT[:, kc, tb * 128:(tb + 1) * 128], rhs=wview_fn(kc),
                                          start=(kc == 0), stop=(kc == KC - 1)), R=[xT, w], W=[pa])
        k.op("act", lambda e: e.activation(out=v[:, :, 0:dv],
                                           in_=pa[:, 0:nh * dv].rearrange("p (h d) -> p h d", h=nh), func=AF.Copy),
             R=[pa], W=[v])
        k.dma("sp", dst.t[tb * 128:(tb + 1) * 128, :, :], v[:], R=[v], W=[dst])


def causal_attn_head(k, ctx, S, dk, dv, QT, KT, V1, scale, accbs, O_d, ocol, fin):
    NQ = min(512, S)
    NS = NQ // 128
    dv1 = dv + 1
    r1, ob = fin
    units = []

    def make_after(qt, accb):
        q0 = qt * NQ
        o = ob[qt % 2]

        def after():
            for s in range(NS):
                a = accb[:, s * dv1:(s + 1) * dv1]
                k.op("dve", lambda e: e.reciprocal(out=r1[:, s:s + 1], in_=a[:, dv:dv1]), R=[accb], W=[r1])
                k.op("dve", lambda e: e.tensor_scalar(out=o[:, s, :], in0=a[:, 0:dv], scalar1=r1[:, s:s + 1],
                                                      scalar2=None, op0=ALU.mult), R=[accb, r1], W=[o])
            k.dma("sp", O_d.t[q0:q0 + NQ, ocol:ocol + dv].rearrange("(s p) d -> p s d", p=128), o[:, 0:NS, :],
                  R=[o], W=[O_d])
        return after

    for qt in range(S // NQ):
        q0 = qt * NQ
        nk = (q0 + NQ) // 128
        accb = accbs[qt % 2]
        accs = [(accb, accb[:, s * dv1:(s + 1) * dv1], s == 0) for s in range(NS)]
        for kt in range(nk):
            units.append(dict(kt_ap=KT[0:dk, kt * 128:(kt + 1) * 128], q_ap=QT[0:dk, q0:q0 + NQ], Rk=[KT], Rq=[QT],
                              scale=scale, v_ap=V1[:, kt, :], Rv=[V1], accs=accs, first=(kt == 0), last=(kt == nk - 1),
                              masks=causal_masks(q0, kt * 128, NQ), nq=NQ,
                              after=(make_after(qt, accb) if kt == nk - 1 else None)))
    run_units(ctx, units)


RMS_EPS = 1e-6


def emit_mla_proj(k, T, xT_d, win_d, qn_d, kvn_d, wuq_d, wuqsw_d, wukv_d, cs16_d, c2_d, s2_d,
                  QT_d, KnT_d, KrT_d, V1_d, psum):
    xT = k.sb([128, 8, T], BF16, "xT")
    k.dma("pool", xT[:], xT_d.t, R=[xT_d], W=[xT])
    win = load_w_bf(k, win_d, 1024, 416, "win")
    wuq = load_w_bf(k, wuq_d, 256, 1536, "wuq")
    wuqsw = load_w_bf(k, wuqsw_d, 256, 512, "wuqsw")
    wukv = load_w_bf(k, wukv_d, 128, 2048, "wukv")
    qn = k.sb([128, 256], F32, "qn")
    kvn = k.sb([128, 128], F32, "kvn")
    k.dma("sp", qn[:], qn_d.t.partition_broadcast(128), R=[qn_d], W=[qn])
    k.dma("sp", kvn[:], kvn_d.t.partition_broadcast(128), R=[kvn_d], W=[kvn])
    c2 = k.sb([128, T], F32, "c2")
    s2 = k.sb([128, T], F32, "s2")
    k.dma("sp", c2[:], c2_d.t, R=[c2_d], W=[c2])
    k.dma("sp", s2[:], s2_d.t, R=[s2_d], W=[s2])
    cs16 = k.sb([128, T // 128, 2, 16], F32, "cs16")
    k.dma("sp", cs16[:], cs16_d.t.rearrange("(b p) a i -> p b a i", p=128), R=[cs16_d], W=[cs16])
    ident_f = make_ident(k, F32, "ident_f")
    eps = k.sb([128, 1], F32, "eps_rms")
    k.op("pool", lambda e: e.memset(eps[:], RMS_EPS), W=[eps])
    cqT = k.sb([128, 2, T], BF16, "cqT")
    ckvT = k.sb([128, 1, T], BF16, "ckvT")
    krT = k.sb([32, T], BF16, "krT")
    c_sb = k.sb([128, 416], F32, "c_sb")
    sq = k.sb([128, 256], F32, "sq")
    ss = k.sb([128, 2], F32, "ss")
    kr = k.sb([128, 32], F32, "kr")
    ktmp = k.sb([128, 2, 16], F32, "ktmp")
    for tb in range(T // 128):
        pa = psum[4]
        for kc in range(8):
            k.op("pe", lambda e: e.matmul(pa[:, 0:416], lhsT=xT[:, kc, tb * 128:(tb + 1) * 128], rhs=win[:, kc, :],
                                          start=(kc == 0), stop=(kc == 7)), R=[xT, win], W=[pa])
        k.op("act", lambda e: e.activation(out=c_sb[:], in_=pa[:, 0:416], func=AF.Copy), R=[pa], W=[c_sb])
        k.op("act", lambda e: e.activation(out=sq[:, 0:256], in_=c_sb[:, 0:256], func=AF.Square, accum_out=ss[:, 0:1]),
             R=[c_sb], W=[sq, ss])
        k.op("act", lambda e: e.activation(out=sq[:, 0:128], in_=c_sb[:, 256:384], func=AF.Square, accum_out=ss[:, 1:2]),
             R=[c_sb, sq, ss], W=[sq, ss])
        k.op("act", lambda e: e.activation(out=ss[:, 0:1], in_=ss[:, 0:1], func=AF.Sqrt, scale=1.0 / 256, bias=eps[:]),
             R=[ss, eps], W=[ss])
        k.op("act", lambda e: e.activation(out=ss[:, 1:2], in_=ss[:, 1:2], func=AF.Sqrt, scale=1.0 / 128, bias=eps[:]),
             R=[ss, eps], W=[ss])
        k.op("dve", lambda e: e.reciprocal(out=ss[:], in_=ss[:]), R=[ss], W=[ss])
        k.op("dve", lambda e: e.scalar_tensor_tensor(out=c_sb[:, 0:256], in0=c_sb[:, 0:256], scalar=ss[:, 0:1],
                                                     in1=qn[:], op0=ALU.mult, op1=ALU.mult), R=[c_sb, ss, qn], W=[c_sb])
        k.op("dve", lambda e: e.scalar_tensor_tensor(out=c_sb[:, 256:384], in0=c_sb[:, 256:384], scalar=ss[:, 1:2],
                                                     in1=kvn[:], op0=ALU.mult, op1=ALU.mult), R=[c_sb, ss, kvn], W=[c_sb])
        cc = cs16[:, tb, 0, :]
        sn = cs16[:, tb, 1, :]
        k.op("dve", lambda e: e.tensor_tensor(out=ktmp[:, 0, :], in0=c_sb[:, 384:400], in1=cc, op=ALU.mult),
             R=[c_sb, cs16], W=[ktmp])
        k.op("dve", lambda e: e.tensor_tensor(out=ktmp[:, 1, :], in0=c_sb[:, 400:416], in1=sn, op=ALU.mult),
             R=[c_sb, cs16, ktmp], W=[ktmp])
        k.op("dve", lambda e: e.tensor_tensor(out=kr[:, 0:16], in0=ktmp[:, 0, :], in1=ktmp[:, 1, :], op=ALU.subtract),
             R=[ktmp], W=[kr])
        k.op("dve", lambda e: e.tensor_tensor(out=ktmp[:, 0, :], in0=c_sb[:, 400:416], in1=cc, op=ALU.mult),
             R=[c_sb, cs16, kr], W=[ktmp])
        k.op("dve", lambda e: e.tensor_tensor(out=ktmp[:, 1, :], in0=c_sb[:, 384:400], in1=sn, op=ALU.mult),
             R=[c_sb, cs16, ktmp], W=[ktmp])
        k.op("dve", lambda e: e.tensor_tensor(out=kr[:, 16:32], in0=ktmp[:, 0, :], in1=ktmp[:, 1, :], op=ALU.add),
             R=[ktmp, kr], W=[kr])
        pt = psum[5]
        for j in range(3):
            k.op("pe", lambda e: e.transpose(out=pt[:, j * 128:(j + 1) * 128], in_=c_sb[:, j * 128:(j + 1) * 128],
                                             identity=ident_f[:]), R=[c_sb, ident_f], W=[pt])
        k.op("pe", lambda e: e.transpose(out=pt[0:32, 384:512], in_=kr[:], identity=ident_f[:]),
             R=[kr, ident_f], W=[pt])
        k.op("act", lambda e: e.activation(out=cqT[:, :, tb * 128:(tb + 1) * 128],
                                           in_=pt[:, 0:256].rearrange("p (j t) -> p j t", j=2), func=AF.Copy),
             R=[pt], W=[cqT])
        k.op("dve", lambda e: e.tensor_copy(out=ckvT[:, 0, tb * 128:(tb + 1) * 128], in_=pt[:, 256:384]),
             R=[pt], W=[ckvT])
        k.op("dve", lambda e: e.tensor_copy(out=krT[:, tb * 128:(tb + 1) * 128], in_=pt[0:32, 384:512]),
             R=[pt], W=[krT])
    k.dma("sp", KrT_d.t, krT[:], R=[krT], W=[KrT_d])
    pc = ProjCtx(k, psum, min(512, T))
    for h in range(16):
        proj_fm(pc, cqT, 2, T, wuq, h * 96, 64, QT_d, lambda ts: QT_d.t[h, 0:64, ts])
        proj_fm(pc, cqT, 2, T, wuq, h * 96 + 64, 32, QT_d, lambda ts: QT_d.t[h, 64:96, ts],
                rope=(wuqsw, h * 32, c2, s2), prow=64)
        proj_fm(pc, ckvT, 1, T, wukv, h * 128, 64, KnT_d, lambda ts: KnT_d.t[h, :, ts])
    wv = wukv[:, 0, :].rearrange("p (h d) -> p h d", h=16)
    for half in range(2):
        pass
    vb = [k.sb([128, 16, 65], BF16, "v1b%d" % i) for i in range(2)]
    for b in vb:
        k.op("pool", lambda e: e.memset(b[:], 1.0), W=[b])
    for tb in range(T // 128):
        v = vb[tb % 2]
        for half in range(2):
            pa = psum[4 + half]
            k.op("pe", lambda e: e.matmul(pa[:], lhsT=ckvT[:, 0, tb * 128:(tb + 1) * 128],
                                          rhs=wv[:, half * 8:(half + 1) * 8, 64:128], start=True, stop=True),
                 R=[ckvT, wukv], W=[pa])
            k.op("act", lambda e: e.activation(out=v[:, half * 8:(half + 1) * 8, 0:64],
                                               in_=pa[:].rearrange("p (h d) -> p h d", h=8), func=AF.Copy),
                 R=[pa], W=[v])
        k.dma("sp", V1_d.t[tb * 128:(tb + 1) * 128, :, :], v[:], R=[v], W=[V1_d])


class ckvT_view:
    def __init__(self, b):
        self.b = b
        self.lw = None

    def __getitem__(self, idx):
        p, kc, ts = idx
        return self.b[p, ts]


def emit_mla_attn(k, S, QT_d, KT_d, V1_d, O_d, psum, nheads=2):
    NKT = S // 128
    ctx = AttnCtx(k, psum[0:4])
    r1 = k.sb([128, 4], F32, "r1")
    ob = [k.sb([128, 4, 64], BF16, "ob%d" % i) for i in range(2)]
    QT = [k.sb([96, S], BF16, "QT%d" % i) for i in range(2)]
    KT = [k.sb([96, S], BF16, "KT%d" % i) for i in range(2)]
    V1 = [k.sb([128, NKT, 65], BF16, "V1%d" % i) for i in range(2)]
    scale = 96.0 ** -0.5
    for h in range(nheads):
        q, kk, v = QT[h % 2], KT[h % 2], V1[h % 2]
        k.dma("sp", kk[:], KT_d.t[h], R=[KT_d], W=[kk])
        k.dma("sp", v[:], V1_d.t[h], R=[V1_d], W=[v])
        k.dma("sp", q[:], QT_d.t[h], R=[QT_d], W=[q])
        causal_attn_head(k, ctx, S, 96, 64, q, kk, v, scale, psum[4 + 2 * (h % 2):6 + 2 * (h % 2)], O_d, h * 64, (r1, ob))


def emit_nsa_proj(k, T, xT_d, w_d, wsw_d, c2_d, s2_d, QT_d, KsT_d, KwT_d, KcT_d, VcT_d, V1s_d, V1w_d, gate_d, psum):
    xT = k.sb([128, 8, T], BF16, "xT")
    k.dma("pool", xT[:], xT_d.t, R=[xT_d], W=[xT])
    w = load_w_bf(k, w_d, 1024, 2608, "w_in")
    wsw = load_w_bf(k, wsw_d, 1024, 1536, "w_sw")
    c2 = k.sb([128, T], F32, "c2")
    s2 = k.sb([128, T], F32, "s2")
    k.dma("sp", c2[:], c2_d.t, R=[c2_d], W=[c2])
    k.dma("sp", s2[:], s2_d.t, R=[s2_d], W=[s2])
    pc = ProjCtx(k, psum, min(512, T))
    for h in range(8):
        proj_fm(pc, xT, 8, T, w, h * 128, 128, QT_d, lambda ts: QT_d.t[h, :, ts], rope=(wsw, h * 128, c2, s2))
    for j in range(2):
        proj_fm(pc, xT, 8, T, w, 1536 + j * 128, 128, KsT_d, lambda ts: KsT_d.t[j, :, ts],
                rope=(wsw, 1024 + j * 128, c2, s2))
        proj_fm(pc, xT, 8, T, w, 2048 + j * 128, 128, KwT_d, lambda ts: KwT_d.t[j, :, ts],
                rope=(wsw, 1280 + j * 128, c2, s2))
        proj_fm(pc, xT, 8, T, w, 1024 + j * 128, 128, KcT_d, lambda ts: KcT_d.t[j, :, ts])
        proj_fm(pc, xT, 8, T, w, 1280 + j * 128, 128, VcT_d, lambda ts: VcT_d.t[j, :, ts])
    proj_tm_v1(k, psum, xT, 8, T, w, lambda kc: w[:, kc, 1792:2048], 4, 64, V1s_d)
    proj_tm_v1(k, psum, xT, 8, T, w, lambda kc: w[:, kc, 2304:2560], 4, 64, V1w_d)
    gt = [k.sb([128, 48], F32, "gt%d" % i) for i in range(2)]
    for tb in range(T // 128):
        pa = psum[6 + tb % 2]
        g = gt[tb % 2]
        for kc in range(8):
            k.op("pe", lambda e: e.matmul(pa[:, 0:48], lhsT=xT[:, kc, tb * 128:(tb + 1) * 128], rhs=w[:, kc, 2560:2608],
                                          start=(kc == 0), stop=(kc == 7)), R=[xT, w], W=[pa])
        k.op("act", lambda e: e.activation(out=g[:], in_=pa[:, 0:48], func=AF.Sigmoid), R=[pa], W=[g])
        k.dma("sp", gate_d.t[tb * 128:(tb + 1) * 128, :], g[:], R=[g], W=[gate_d])


def emit_nsa_compress(k, NB, win_k_d, win_v_d, posk_d, posv_d, w1k_d, w1v_d, w2k_d, w2ksw_d, w2v_d, c2_d, s2_d,
                      KcT_d, V1c_d, psum):
    W = 16 * NB + 16
    ident_f = make_ident(k, F32, "ident_f")
    c2 = k.sb([64, NB], F32, "c2c")
    s2 = k.sb([64, NB], F32, "s2c")
    k.dma("sp", c2[:], c2_d.t, R=[c2_d], W=[c2])
    k.dma("sp", s2[:], s2_d.t, R=[s2_d], W=[s2])
    v1 = k.sb([128, 65], BF16, "v1c")
    k.op("pool", lambda e: e.memset(v1[:], 1.0), W=[v1])
    for kv, (win_d, pos_d, w1_d, w2_d) in enumerate(((win_k_d, posk_d, w1k_d, w2k_d), (win_v_d, posv_d, w1v_d, w2v_d))):
        win = k.sb([64, 4, W], BF16, "win%d" % kv)
        k.dma("sp", win[:], win_d.t.rearrange("g d w -> d g w"), R=[win_d], W=[win])
        w1 = k.sb([64, 32, 256], BF16, "w1_%d" % kv)
        k.dma("pool", w1[:], w1_d.t.rearrange("(l d) h -> d l h", d=64), R=[w1_d], W=[w1])
        w2 = k.sb([128, 2, 64], BF16, "w2_%d" % kv)
        k.dma("pool", w2[:], w2_d.t.rearrange("(c p) d -> p c d", p=128), R=[w2_d], W=[w2])
        if kv == 0:
            w2sw = k.sb([128, 2, 64], BF16, "w2sw")
            k.dma("pool", w2sw[:], w2ksw_d.t.rearrange("(c p) d -> p c d", p=128), R=[w2ksw_d], W=[w2sw])
        pos = k.sb([32, 64], F32, "pos%d" % kv)
        k.dma("sp", pos[:], pos_d.t, R=[pos_d], W=[pos])
        posT = k.sb([64, 32], BF16, "posT%d" % kv)
        pm = psum[7]
        k.op("pe", lambda e: e.transpose(out=pm[0:64, 0:32], in_=pos[:], identity=ident_f[0:32, 0:32]),
             R=[pos, ident_f], W=[pm])
        k.op("act", lambda e: e.activation(out=posT[:], in_=pm[0:64, 0:32], func=AF.Copy), R=[pm], W=[posT])
        pbias = k.sb([128, 2], F32, "pbias%d" % kv)
        for hc in range(2):
            for l in range(32):
                k.op("pe", lambda e: e.matmul(pm[:, 64 + hc:65 + hc], lhsT=w1[:, l, hc * 128:(hc + 1) * 128],
                                              rhs=posT[:, l:l + 1], start=(l == 0), stop=(l == 31)),
                     R=[w1, posT], W=[pm])
        k.op("dve", lambda e: e.tensor_copy(out=pbias[:], in_=pm[:, 64:66]), R=[pm], W=[pbias])
        x = k.sb([128, NB], F32, "gx%d" % kv)
        x2 = k.sb([128, NB], F32, "gx2%d" % kv)
        hT = [k.sb([128, NB], BF16, "hT%d_%d" % (kv, i)) for i in range(2)]
        a1 = k.sb([64, NB], F32, "ca1_%d" % kv)
        a2 = k.sb([64, NB], F32, "ca2_%d" % kv)
        ko = k.sb([64, NB], BF16, "ko%d" % kv)
        for g in range(4):
            for hc in range(2):
                ph = psum[hc]
                for l in range(32):
                    k.op("pe", lambda e: e.matmul(ph[:, 0:NB], lhsT=w1[:, l, hc * 128:(hc + 1) * 128],
                                                  rhs=win[:, g, l:l + 16 * (NB - 1) + 1:16], start=(l == 0), stop=(l == 31)),
                         R=[w1, win], W=[ph])
                k.op("act", lambda e: e.activation(out=x[:], in_=ph[:, 0:NB], func=AF.Identity,
                                                   bias=pbias[:, hc:hc + 1], scale=1.0), R=[ph, pbias], W=[x])
                k.op("dve", lambda e: e.tensor_tensor(out=x2[:], in0=x[:], in1=x[:], op=ALU.mult), R=[x], W=[x2])
                k.op("dve", lambda e: e.tensor_scalar(out=x2[:], in0=x2[:], scalar1=0.044715, scalar2=1.0,
                                                      op0=ALU.mult, op1=ALU.add), R=[x2], W=[x2])
                k.op("dve", lambda e: e.tensor_tensor(out=x2[:], in0=x2[:], in1=x[:], op=ALU.mult), R=[x2, x], W=[x2])
                k.op("act", lambda e: e.activation(out=x2[:], in_=x2[:], func=AF.Sigmoid, scale=1.5957691216057308),
                     R=[x2], W=[x2])
                k.op("dve", lambda e: e.tensor_tensor(out=hT[hc][:], in0=x[:], in1=x2[:], op=ALU.mult),
                     R=[x, x2], W=[hT[hc]])
            if kv == 0:
                pa, pb = psum[2], psum[3]
                for hc in range(2):
                    k.op("pe", lambda e: e.matmul(pa[0:64, 0:NB], lhsT=w2[:, hc, :], rhs=hT[hc][:],
                                                  start=(hc == 0), stop=(hc == 1)), R=[w2, hT[hc]], W=[pa])
                for hc in range(2):
                    k.op("pe", lambda e: e.matmul(pb[0:64, 0:NB], lhsT=w2sw[:, hc, :], rhs=hT[hc][:],
                                                  start=(hc == 0), stop=(hc == 1)), R=[w2sw, hT[hc]], W=[pb])
                k.op("dve", lambda e: e.tensor_tensor(out=a1[:], in0=pa[0:64, 0:NB], in1=c2[:], op=ALU.mult),
                     R=[pa, c2], W=[a1])
                k.op("dve", lambda e: e.tensor_tensor(out=a2[:], in0=pb[0:64, 0:NB], in1=s2[:], op=ALU.mult),
                     R=[pb, s2], W=[a2])
                k.op("dve", lambda e: e.tensor_tensor(out=ko[:], in0=a1[:], in1=a2[:], op=ALU.add), R=[a1, a2], W=[ko])
                k.dma("sp", KcT_d.t[g], ko[:], R=[ko], W=[KcT_d])
            else:
                pa = psum[2]
                for hc in range(2):
                    k.op("pe", lambda e: e.matmul(pa[0:NB, 0:64], lhsT=hT[hc][:], rhs=w2[:, hc, :],
                                                  start=(hc == 0), stop=(hc == 1)), R=[w2, hT[hc]], W=[pa])
                k.op("act", lambda e: e.activation(out=v1[0:NB, 0:64], in_=pa[0:NB, 0:64], func=AF.Copy), R=[pa], W=[v1])
                k.dma("sp", V1c_d.t[g], v1[0:NB, :], R=[v1], W=[V1c_d])


def emit_nsa_attn(k, S, QT4_d, Qdup_d, KcT2_d, V1cA_d, KsKw_d, V1s_d, V1w_d, gate_d, O_d, oc_d, psum):
    NQ = min(512, S)
    NQT = S // NQ
    NS = NQ // 128
    NKT = S // 128
    NSLC = S // 64
    NJC = (NSLC + 127) // 128
    JW = min(128, NSLC)
    NC = ((S - 32) // 16 + 1 + 127) // 128 * 128
    NNT = NC // 128
    CW = 65 + NSLC
    scale = 0.125
    ident_b = make_ident(k, BF16, "ident_b")
    biasT = k.sb([128, NJC, S], BF16, "biasT")
    KcT2 = k.sb([128, NC], BF16, "KcT2")
    k.dma("sp", KcT2[:], KcT2_d.t, R=[KcT2_d], W=[KcT2])
    V1cA = k.sb([128, NNT, CW], BF16, "V1cA")
    k.dma("sp", V1cA[:], V1cA_d.t, R=[V1cA_d], W=[V1cA])
    gate = k.sb([128, NKT, 2, 3], F32, "gate")
    k.dma("sp", gate[:], gate_d.t, R=[gate_d], W=[gate])
    ctx = AttnCtx(k, psum[0:2])
    accb = psum[2:6]
    pmisc = psum[6]
    ptr = psum[7]
    oct = [k.sb([128, 4, 2, 64], BF16, "oct%d" % i) for i in range(2)]
    tiny = 1e-30
    k.push_scope()
    QT4 = k.sb([128, 2, S], BF16, "QT4")
    for p in range(2):
        k.dma("sp", QT4[:, p, :], QT4_d.t[p], R=[QT4_d], W=[QT4])
    imp = k.sb([128, 4, NSLC], F32, "imp")
    rl = k.sb([128, 1], F32, "rl")
    m8a = k.sb([128, 8], F32, "m8a")
    m8b = k.sb([128, 8], F32, "m8b")
    sc2 = k.sb([128, NSLC], F32, "sc2")
    bsel = k.sb([128, NJC * 128], BF16, "bsel")
    if NSLC < 128:
        k.op("pool", lambda e: e.memset(bsel[:], 0.0), W=[bsel])
    ctx.sc = [psum[0], psum[1], psum[6]]

    def fin_head(qt, r, has):
        oc = oct[qt % 2]

        def after():
            for s in range(NS):
                a = accb[s]
                if has:
                    k.op("dve", lambda e: e.tensor_scalar(out=rl[:], in0=a[:, 64:65], scalar1=tiny, scalar2=None,
                                                          op0=ALU.max), R=[a], W=[rl])
                    k.op("dve", lambda e: e.reciprocal(out=rl[:], in_=rl[:]), R=[rl], W=[rl])
                    if r == 0:
                        k.op("dve", lambda e: e.tensor_scalar(out=imp[:, s, :], in0=a[:, 65:CW], scalar1=rl[:],
                                                              scalar2=None, op0=ALU.mult), R=[a, rl], W=[imp])
                    else:
                        k.op("dve", lambda e: e.scalar_tensor_tensor(out=imp[:, s, :], in0=a[:, 65:CW], scalar=rl[:],
                                                                     in1=imp[:, s, :], op0=ALU.mult, op1=ALU.add),
                             R=[a, rl, imp], W=[imp])
                    if r < 2:
                        k.op("dve", lambda e: e.tensor_scalar(out=oc[:, s, r, :], in0=a[:, 0:64], scalar1=rl[:],
                                                              scalar2=None, op0=ALU.mult), R=[a, rl], W=[oc])
            if r == 3:
                fin_qtile(qt)
        return after

    def fin_qtile(qt):
        q0 = qt * NQ
        oc = oct[qt % 2]
        k.dma("sp", oc_d.t[q0:q0 + NQ, :].rearrange("(s p) (r d) -> p s r d", p=128, r=2), oc[:, 0:NS, :, :],
              R=[oc], W=[oc_d])
        for s in range(NS):
            qb = q0 + s * 128
            sc = imp[:, s, :]
            k.op("pool", lambda e: e.affine_select(out=sc, in_=sc, pattern=[[-64, NSLC]], compare_op=ALU.is_ge,
                                                   fill=k.fillreg(1e9), base=qb - 128, channel_multiplier=1),
                 R=[imp], W=[imp])
            k.op("pool", lambda e: e.memset(imp[:, s, 0:1], 1e9), R=[imp], W=[imp])
            k.op("pool", lambda e: e.affine_select(out=sc, in_=sc, pattern=[[-64, NSLC]], compare_op=ALU.is_ge,
                                                   fill=k.fillreg(-1.0), base=qb, channel_multiplier=1),
                 R=[imp], W=[imp])
            k.op("dve", lambda e: e.max(out=m8a[:], in_=sc), R=[imp], W=[m8a])
            k.op("dve", lambda e: e.match_replace(out=sc2[:], in_to_replace=m8a[:], in_values=sc, imm_value=-3e38),
                 R=[imp, m8a], W=[sc2])
            k.op("dve", lambda e: e.max(out=m8b[:], in_=sc2[:]), R=[sc2], W=[m8b])
            k.op("dve", lambda e: e.tensor_scalar(out=sc2[:], in0=sc, scalar1=m8b[:, 7:8], scalar2=1.0,
                                                  op0=ALU.is_ge, op1=ALU.subtract), R=[imp, m8b, sc2], W=[sc2])
            k.op("dve", lambda e: e.tensor_scalar(out=bsel[:, 0:NSLC], in0=sc2[:], scalar1=30000.0, scalar2=None,
                                                  op0=ALU.mult), R=[sc2], W=[bsel])
            pv = ptr[:].bitcast(BF16)
            for c in range(NJC):
                k.op("pe", lambda e: e.transpose(out=pv[:, c * 128:(c + 1) * 128], in_=bsel[:, c * 128:(c + 1) * 128],
                                                 identity=ident_b[:]), R=[bsel, ident_b], W=[ptr])
                k.op("act", lambda e: e.activation(out=biasT[:, c, qb:qb + 128], in_=pv[:, c * 128:(c + 1) * 128],
                                                   func=AF.Copy), R=[ptr], W=[biasT])

    units = []
    for qt in range(NQT):
        q0 = qt * NQ
        nts = [nt for nt in range(NNT) if 16 * (128 * nt) + 31 <= q0 + NQ - 1]
        assert nts
        for r in range(4):
            pr = slice((r % 2) * 64, (r % 2) * 64 + 64)
            accs = [(accb[s], accb[s][:, 0:CW], True) for s in range(NS)]
            for i, nt in enumerate(nts):
                n0 = nt * 128
                fully = (16 * (n0 + 127) + 31 <= q0)
                masks = [] if fully else [dict(base=q0 - 16 * n0 - 31, cm=-16, step=1)]
                units.append(dict(kt_ap=KcT2[pr, n0:n0 + 128], q_ap=QT4[pr, r // 2, q0:q0 + NQ], Rk=[KcT2], Rq=[QT4],
                                  scale=scale, v_ap=V1cA[:, nt, :], Rv=[V1cA], accs=accs, first=(i == 0),
                                  last=(i == len(nts) - 1), masks=masks, nq=NQ,
                                  after=(fin_head(qt, r, True) if i == len(nts) - 1 else None)))
    run_units(ctx, units)
    k.pop_scope()
    k.push_scope()
    Ibig = k.sb([128, 8192], BF16, "Ibig")
    k.op("pool", lambda e: e.memset(Ibig[:], 1.0), W=[Ibig])
    k.op("pool", lambda e: e.affine_select(out=Ibig[:], in_=Ibig[:], pattern=[[1, 8192]], compare_op=ALU.is_ge,
                                           fill=k.fillreg(0.0), base=0, channel_multiplier=-64), R=[Ibig], W=[Ibig])
    k.op("pool", lambda e: e.affine_select(out=Ibig[:], in_=Ibig[:], pattern=[[-1, 8192]], compare_op=ALU.is_ge,
                                           fill=k.fillreg(0.0), base=63, channel_multiplier=64), R=[Ibig], W=[Ibig])
    KsKw = k.sb([128, S], BF16, "KsKw")
    k.dma("sp", KsKw[:], KsKw_d.t, R=[KsKw_d], W=[KsKw])
    V1s = k.sb([128, NKT, 65], BF16, "V1s")
    V1wr = [k.sb([128, 8, 65], BF16, "V1w%d" % i) for i in range(2)]
    ocr = [k.sb([128, 4, 64], BF16, "ocr%d" % i) for i in range(2)]
    k.dma("sp", V1s[:], V1s_d.t, R=[V1s_d], W=[V1s])
    Qd = k.sb([128, S], BF16, "Qd")
    ctx2 = AttnCtx(k, psum[0:4], n_pt=4)
    accS, accW = psum[4], psum[5]
    rs = k.sb([128, 1], F32, "rs")
    rw = k.sb([128, 1], F32, "rw")
    of = k.sb([128, 64], F32, "of")
    ob = [k.sb([128, 4, 64], BF16, "nob%d" % i) for i in range(2)]
    oi = 0
    accSs, accWs = [psum[4], psum[6]], [psum[5], psum[7]]
    state = {"oi": 0}
    for r in range(2):
        k.dma("sp", Qd[:], Qdup_d.t[r], R=[Qdup_d], W=[Qd])
        units = []
        for qt in range(NQT):
            q0 = qt * NQ
            nk = (q0 + NQ) // 128
            accS, accW = accSs[qt % 2], accWs[qt % 2]
            kts = [kt for kt in range(nk) if kt * 128 + 127 > q0 - 512]
            V1w = V1wr[qt % 2]
            oc = ocr[qt % 2]
            o = ob[qt % 2]

            def pre(V1w=V1w, oc=oc, kts=kts, q0=q0):
                k.dma("sp", V1w[:, 0:len(kts), :], V1w_d.t[:, kts[0]:kts[0] + len(kts), :], R=[V1w_d], W=[V1w])
                k.dma("sp", oc[:, 0:NS, :], oc_d.t[q0:q0 + NQ, r * 64:(r + 1) * 64].rearrange("(s p) d -> p s d", p=128),
                      R=[oc_d], W=[oc])

            def after(accS=accS, accW=accW, oc=oc, o=o, qt=qt, q0=q0):
                for s in range(NS):
                    qb = qt * NS + s
                    aS = accS[:, s * 65:(s + 1) * 65]
                    aW = accW[:, s * 65:(s + 1) * 65]
                    k.op("dve", lambda e: e.reciprocal(out=rs[:], in_=aS[:, 64:65]), R=[accS], W=[rs])
                    k.op("dve", lambda e: e.reciprocal(out=rw[:], in_=aW[:, 64:65]), R=[accW], W=[rw])
                    k.op("dve", lambda e: e.tensor_tensor(out=rs[:], in0=rs[:], in1=gate[:, qb, r, 1:2], op=ALU.mult),
                         R=[rs, gate], W=[rs])
                    k.op("dve", lambda e: e.tensor_tensor(out=rw[:], in0=rw[:], in1=gate[:, qb, r, 2:3], op=ALU.mult),
                         R=[rw, gate], W=[rw])
                    k.op("dve", lambda e: e.tensor_scalar(out=of[:], in0=oc[:, s, :], scalar1=gate[:, qb, r, 0:1],
                                                          scalar2=None, op0=ALU.mult), R=[oc, gate], W=[of])
                    k.op("dve", lambda e: e.scalar_tensor_tensor(out=of[:], in0=aS[:, 0:64], scalar=rs[:], in1=of[:],
                                                                 op0=ALU.mult, op1=ALU.add), R=[accS, rs, of], W=[of])
                    k.op("dve", lambda e: e.scalar_tensor_tensor(out=o[:, s, :], in0=aW[:, 0:64], scalar=rw[:], in1=of[:],
                                                                 op0=ALU.mult, op1=ALU.add), R=[accW, rw, of], W=[o])
                k.dma("sp", O_d.t[q0:q0 + NQ, r * 64:(r + 1) * 64].rearrange("(s p) d -> p s d", p=128), o[:, 0:NS, :],
                      R=[o], W=[O_d])

            accs = [(accS, accS[:, s * 65:(s + 1) * 65], s == 0) for s in range(NS)]
            for kt in range(nk):
                c = (kt * 2) // 128
                ktl = kt - c * 64
                units.append(dict(kt_ap=KsKw[0:64, kt * 128:(kt + 1) * 128], q_ap=Qd[0:64, q0:q0 + NQ], Rk=[KsKw],
                                  Rq=[Qd], scale=scale, v_ap=V1s[:, kt, :], Rv=[V1s], accs=accs, first=(kt == 0),
                                  last=(kt == nk - 1), masks=causal_masks(q0, kt * 128, NQ), nq=NQ,
                                  pre=(pre if kt == 0 else None),
                                  bias_mm=(Ibig[0:JW, ktl * 128:(ktl + 1) * 128], biasT[0:JW, c, q0:q0 + NQ],
                                           [Ibig, biasT])))
            accs = [(accW, accW[:, s * 65:(s + 1) * 65], s == 0) for s in range(NS)]
            for i, kt in enumerate(kts):
                k0 = kt * 128
                masks = causal_masks(q0, k0, NQ)
                if k0 < q0:
                    masks = masks + [dict(base=k0 - q0 + 511, cm=1, step=-1)]
                units.append(dict(kt_ap=KsKw[64:128, k0:k0 + 128], q_ap=Qd[64:128, q0:q0 + NQ], Rk=[KsKw], Rq=[Qd],
                                  scale=scale, v_ap=V1w[:, i, :], Rv=[V1w], accs=accs, first=(i == 0),
                                  last=(i == len(kts) - 1), masks=masks, nq=NQ,
                                  after=(after if i == len(kts) - 1 else None)))
        run_units(ctx2, units)
    k.pop_scope()


import math
import ml_dtypes
from concourse.bass_utils import run_bass_kernel_spmd

NCORES = 8
BF = ml_dtypes.bfloat16


def _rope_tables(pos, dim):
    inv = (np.float32(10000.0) ** (-np.arange(0, dim, 2, dtype=np.float32) / np.float32(dim))).astype(np.float32)
    ang = pos.astype(np.float32)[:, None] * inv[None, :]
    return np.cos(ang).astype(np.float32), np.sin(ang).astype(np.float32)


def _launch(build, in_maps):
    k = KB()
    outs = build(k)
    k.finish(outs)
    k.close()
    res = run_bass_kernel_spmd(k.nc, in_maps, core_ids=list(range(len(in_maps))))
    return res.results


def _psum(k):
    return [k.ps([128, 512], F32, "pb%d" % i) for i in range(8)]


def _xT_shards(h, S):
    T = S // NCORES
    out = []
    for c in range(NCORES):
        out.append(np.ascontiguousarray(h[c * T:(c + 1) * T].T.reshape(8, 128, T).transpose(1, 0, 2)))
    return out


def _swap_cols(w, nheads, d):
    half = d // 2
    idx = np.concatenate([np.concatenate([np.arange(hh * d + half, hh * d + d), np.arange(hh * d, hh * d + half)])
                          for hh in range(nheads)])
    return np.ascontiguousarray(w[:, idx])


def _tables64(pos):
    cos, sin = _rope_tables(pos, 64)
    c2 = np.ascontiguousarray(np.tile(cos.T, (4, 1)))
    s2 = np.ascontiguousarray(np.tile(np.concatenate([-sin.T, sin.T], 0), (2, 1)))
    return c2, s2


def _OT_shards(O_all, S):
    T = S // NCORES
    return [np.ascontiguousarray(O_all[c * T:(c + 1) * T].T.reshape(8, 128, T).transpose(1, 0, 2)) for c in range(NCORES)]


def _v1_head_layout(v, S):
    return np.ascontiguousarray(v.reshape(S // 128, 128, v.shape[-1]).transpose(1, 0, 2))


def run_post(S, O_all, h, i, P):
    T = S // NCORES
    NT = T // 128
    d = {}

    def build(k):
        OTd = k.dram("OTd", [128, 8, T], BF16, "ExternalInput")
        h_d = k.dram("h", [T, 1024], F32, "ExternalInput")
        wout_d = k.dram("wout", [1024, 1024], F32, "ExternalInput")
        v = {n: k.dram(n, [1024], F32, "ExternalInput") for n in ("ln1g", "ln1b", "ln2g", "ln2b")}
        wr_d = k.dram("wr", [1024, 32], F32, "ExternalInput")
        br_d = k.dram("br", [32], F32, "ExternalInput")
        wgu_d = k.dram("wgu", [32, 1024, 2048], F32, "ExternalInput")
        bgu_d = k.dram("bgu", [32, 2048], F32, "ExternalInput")
        wd_d = k.dram("wd", [32, 1024, 1024], F32, "ExternalInput")
        bd_d = k.dram("bd", [32, 1024], F32, "ExternalInput")
        out_d = k.dram("out", [T, 1024], F32, "ExternalOutput")
        XT = k.sb([128, 8, T], BF16, "XT")
        k.dma("sp", XT[:], OTd.t, R=[OTd], W=[XT])
        emit_post(k, NT, XT, h_d, wout_d, v["ln1g"], v["ln1b"], v["ln2g"], v["ln2b"], wr_d, br_d, wgu_d, bgu_d,
                  wd_d, bd_d, out_d)
        return [out_d]

    OTs = _OT_shards(O_all, S)
    common = dict(wout=P["wout"], ln1g=P["ln1_g"][i], ln1b=P["ln1_b"][i], ln2g=P["ln2_g"][i], ln2b=P["ln2_b"][i],
                  wr=P["moe_w_router"][i], br=P["moe_b_router"][i], wgu=P["moe_w_gu"][i], bgu=P["moe_b_gu"][i],
                  wd=P["moe_w_down"][i], bd=P["moe_b_down"][i])
    ins = [dict(OTd=OTs[c], h=np.ascontiguousarray(h[c * T:(c + 1) * T]), **common) for c in range(NCORES)]
    res = _launch(build, ins)
    return np.concatenate([r["out"] for r in res], 0)


def run_da(S, h, i, j, P):
    T = S // NCORES
    lam_init = 0.8 - 0.6 * math.exp(-0.3 * i)

    def buildA(k):
        xT_d = k.dram("xT", [128, 8, T], F32, "ExternalInput")
        w_d = k.dram("w", [1024, 3072], F32, "ExternalInput")
        wsw_d = k.dram("wsw", [1024, 2048], F32, "ExternalInput")
        c2_d = k.dram("c2", [128, T], F32, "ExternalInput")
        s2_d = k.dram("s2", [128, T], F32, "ExternalInput")
        QT_d = k.dram("QT", [8, 128, T], BF16, "ExternalOutput")
        KT_d = k.dram("KT", [8, 128, T], BF16, "ExternalOutput")
        V1_d = k.dram("V1", [T, 8, 129], BF16, "ExternalOutput")
        emit_da_proj(k, T, xT_d, w_d, wsw_d, c2_d, s2_d, QT_d, KT_d, V1_d, _psum(k))
        return [QT_d, KT_d, V1_d]

    w = P["da_w_in"][j]
    wsw = _swap_cols(w[:, :2048], 32, 64)
    xTs = _xT_shards(h, S)
    ins = []
    for c in range(NCORES):
        c2, s2 = _tables64(np.arange(c * T, (c + 1) * T))
        ins.append(dict(xT=xTs[c], w=w, wsw=wsw, c2=c2, s2=s2))
    rA = _launch(buildA, ins)
    QT = np.concatenate([r["QT"] for r in rA], 2)
    KT = np.concatenate([r["KT"] for r in rA], 2)
    V1 = np.concatenate([r["V1"] for r in rA], 0)

    def buildB(k):
        QTh = k.dram("QTh", [128, S], BF16, "ExternalInput")
        KTh = k.dram("KTh", [128, S], BF16, "ExternalInput")
        V1h = k.dram("V1h", [128, S // 128, 129], BF16, "ExternalInput")
        ls = [k.dram(n, [64], F32, "ExternalInput") for n in ("lq1", "lk1", "lq2", "lk2")]
        g_d = k.dram("g", [128], F32, "ExternalInput")
        O_d = k.dram("O", [S, 128], BF16, "ExternalOutput")
        emit_da_attn(k, S, QTh, KTh, V1h, ls[0], ls[1], ls[2], ls[3], g_d, O_d, lam_init, _psum(k))
        return [O_d]

    ins = []
    for hd in range(8):
        ins.append(dict(QTh=np.ascontiguousarray(QT[hd]), KTh=np.ascontiguousarray(KT[hd]),
                        V1h=_v1_head_layout(V1[:, hd, :], S), lq1=P["da_lambda_q1"][j], lk1=P["da_lambda_k1"][j],
                        lq2=P["da_lambda_q2"][j], lk2=P["da_lambda_k2"][j], g=P["da_subln"][j]))
    rB = _launch(buildB, ins)
    O_all = np.concatenate([r["O"] for r in rB], 1)
    return run_post(S, O_all, h, i, dict(P, wout=P["da_w_out"][j]))


def run_mla(S, h, i, j, P):
    T = S // NCORES

    def buildA(k):
        xT_d = k.dram("xT", [128, 8, T], F32, "ExternalInput")
        win_d = k.dram("win", [1024, 416], F32, "ExternalInput")
        qn_d = k.dram("qn", [256], F32, "ExternalInput")
        kvn_d = k.dram("kvn", [128], F32, "ExternalInput")
        wuq_d = k.dram("wuq", [256, 1536], F32, "ExternalInput")
        wuqsw_d = k.dram("wuqsw", [256, 512], F32, "ExternalInput")
        wukv_d = k.dram("wukv", [128, 2048], F32, "ExternalInput")
        cs16_d = k.dram("cs16", [T, 2, 16], F32, "ExternalInput")
        c2_d = k.dram("c2", [128, T], F32, "ExternalInput")
        s2_d = k.dram("s2", [128, T], F32, "ExternalInput")
        QT_d = k.dram("QT", [16, 96, T], BF16, "ExternalOutput")
        KnT_d = k.dram("KnT", [16, 64, T], BF16, "ExternalOutput")
        KrT_d = k.dram("KrT", [32, T], BF16, "ExternalOutput")
        V1_d = k.dram("V1", [T, 16, 65], BF16, "ExternalOutput")
        emit_mla_proj(k, T, xT_d, win_d, qn_d, kvn_d, wuq_d, wuqsw_d, wukv_d, cs16_d, c2_d, s2_d,
                      QT_d, KnT_d, KrT_d, V1_d, _psum(k))
        return [QT_d, KnT_d, KrT_d, V1_d]

    wuq = P["mla_w_uq"][j]
    idx = np.concatenate([np.concatenate([np.arange(hh * 96 + 80, hh * 96 + 96), np.arange(hh * 96 + 64, hh * 96 + 80)])
                          for hh in range(16)])
    wuqsw = np.ascontiguousarray(wuq[:, idx])
    xTs = _xT_shards(h, S)
    ins = []
    for c in range(NCORES):
        cos, sin = _rope_tables(np.arange(c * T, (c + 1) * T), 32)
        c2 = np.zeros((128, T), np.float32)
        s2 = np.zeros((128, T), np.float32)
        c2[64:96] = np.concatenate([cos.T, cos.T], 0)
        s2[64:96] = np.concatenate([-sin.T, sin.T], 0)
        cs16 = np.ascontiguousarray(np.stack([cos, sin], 1))
        ins.append(dict(xT=xTs[c], win=P["mla_w_in"][j], qn=P["mla_q_norm"][j], kvn=P["mla_kv_norm"][j], wuq=wuq,
                        wuqsw=wuqsw, wukv=P["mla_w_ukv"][j], cs16=cs16, c2=c2, s2=s2))
    rA = _launch(buildA, ins)
    QT = np.concatenate([r["QT"] for r in rA], 2)
    KnT = np.concatenate([r["KnT"] for r in rA], 2)
    KrT = np.concatenate([r["KrT"] for r in rA], 1)
    V1 = np.concatenate([r["V1"] for r in rA], 0)

    def buildB(k):
        QT_d = k.dram("QTh", [2, 96, S], BF16, "ExternalInput")
        KT_d = k.dram("KTh", [2, 96, S], BF16, "ExternalInput")
        V1_d = k.dram("V1h", [2, 128, S // 128, 65], BF16, "ExternalInput")
        O_d = k.dram("O", [S, 128], BF16, "ExternalOutput")
        emit_mla_attn(k, S, QT_d, KT_d, V1_d, O_d, _psum(k))
        return [O_d]

    ins = []
    for c in range(NCORES):
        hs = (2 * c, 2 * c + 1)
        ins.append(dict(QTh=np.ascontiguousarray(QT[list(hs)]),
                        KTh=np.ascontiguousarray(np.stack([np.concatenate([KnT[hh], KrT], 0) for hh in hs], 0)),
                        V1h=np.stack([_v1_head_layout(V1[:, hh, :], S) for hh in hs], 0)))
    rB = _launch(buildB, ins)
    O_all = np.concatenate([r["O"] for r in rB], 1)
    return run_post(S, O_all, h, i, dict(P, wout=P["mla_w_out"][j]))


def run_nsa(S, h, i, j, P):
    T = S // NCORES
    NC = S // 16
    NB = NC // NCORES
    NSLC = S // 64
    W = 16 * NB + 16

    def buildA(k):
        xT_d = k.dram("xT", [128, 8, T], F32, "ExternalInput")
        w_d = k.dram("w", [1024, 2608], F32, "ExternalInput")
        wsw_d = k.dram("wsw", [1024, 1536], F32, "ExternalInput")
        c2_d = k.dram("c2", [128, T], F32, "ExternalInput")
        s2_d = k.dram("s2", [128, T], F32, "ExternalInput")
        QT_d = k.dram("QT", [8, 128, T], BF16, "ExternalOutput")
        o2 = {n: k.dram(n, [2, 128, T], BF16, "ExternalOutput") for n in ("KsT", "KwT", "KcT", "VcT")}
        V1s_d = k.dram("V1s", [T, 4, 65], BF16, "ExternalOutput")
        V1w_d = k.dram("V1w", [T, 4, 65], BF16, "ExternalOutput")
        gate_d = k.dram("gate", [T, 48], F32, "ExternalOutput")
        emit_nsa_proj(k, T, xT_d, w_d, wsw_d, c2_d, s2_d, QT_d, o2["KsT"], o2["KwT"], o2["KcT"], o2["VcT"],
                      V1s_d, V1w_d, gate_d, _psum(k))
        return [QT_d, V1s_d, V1w_d, gate_d] + list(o2.values())

    w = P["nsa_w_in"][j]
    wsw = np.concatenate([_swap_cols(w[:, 0:1024], 16, 64), _swap_cols(w[:, 1536:1792], 4, 64),
                          _swap_cols(w[:, 2048:2304], 4, 64)], 1)
    xTs = _xT_shards(h, S)
    ins = []
    for c in range(NCORES):
        c2, s2 = _tables64(np.arange(c * T, (c + 1) * T))
        ins.append(dict(xT=xTs[c], w=w, wsw=np.ascontiguousarray(wsw), c2=c2, s2=s2))
    rA = _launch(buildA, ins)
    cat = lambda n, ax: np.concatenate([r[n] for r in rA], ax)
    QT = cat("QT", 2)
    KsT = cat("KsT", 2).reshape(4, 64, S)
    KwT = cat("KwT", 2).reshape(4, 64, S)
    KcT = cat("KcT", 2).reshape(4, 64, S)
    VcT = cat("VcT", 2).reshape(4, 64, S)
    V1s = cat("V1s", 0)
    V1w = cat("V1w", 0)
    gate = cat("gate", 0)

    def buildA2(k):
        wk = k.dram("win_k", [4, 64, W], BF16, "ExternalInput")
        wv = k.dram("win_v", [4, 64, W], BF16, "ExternalInput")
        pk = k.dram("posk", [32, 64], F32, "ExternalInput")
        pv = k.dram("posv", [32, 64], F32, "ExternalInput")
        w1k = k.dram("w1k", [2048, 256], F32, "ExternalInput")
        w1v = k.dram("w1v", [2048, 256], F32, "ExternalInput")
        w2k = k.dram("w2k", [256, 64], F32, "ExternalInput")
        w2ksw = k.dram("w2ksw", [256, 64], F32, "ExternalInput")
        w2v = k.dram("w2v", [256, 64], F32, "ExternalInput")
        c2_d = k.dram("c2", [64, NB], F32, "ExternalInput")
        s2_d = k.dram("s2", [64, NB], F32, "ExternalInput")
        KcT_d = k.dram("KcTo", [4, 64, NB], BF16, "ExternalOutput")
        V1c_d = k.dram("V1co", [4, NB, 65], BF16, "ExternalOutput")
        emit_nsa_compress(k, NB, wk, wv, pk, pv, w1k, w1v, w2k, w2ksw, w2v, c2_d, s2_d, KcT_d, V1c_d, _psum(k))
        return [KcT_d, V1c_d]

    KcP = np.concatenate([KcT, np.zeros((4, 64, 64), KcT.dtype)], 2)
    VcP = np.concatenate([VcT, np.zeros((4, 64, 64), VcT.dtype)], 2)
    w2k = P["nsa_cmp_k_w2"][j]
    ins = []
    for c in range(NCORES):
        n0 = c * NB
        cos, sin = _rope_tables(np.arange(n0, n0 + NB) * 16 + 31, 64)
        c2 = np.ascontiguousarray(np.concatenate([cos.T, cos.T], 0))
        s2 = np.ascontiguousarray(np.concatenate([-sin.T, sin.T], 0))
        ins.append(dict(win_k=np.ascontiguousarray(KcP[:, :, 16 * n0:16 * n0 + W]),
                        win_v=np.ascontiguousarray(VcP[:, :, 16 * n0:16 * n0 + W]),
                        posk=P["nsa_cmp_pos_k"][j], posv=P["nsa_cmp_pos_v"][j], w1k=P["nsa_cmp_k_w1"][j],
                        w1v=P["nsa_cmp_v_w1"][j], w2k=w2k, w2ksw=_swap_cols(w2k, 1, 64), w2v=P["nsa_cmp_v_w2"][j],
                        c2=c2, s2=s2))
    rA2 = _launch(buildA2, ins)
    Kc = np.concatenate([r["KcTo"] for r in rA2], 2)
    V1c = np.concatenate([r["V1co"] for r in rA2], 1)
    A = np.zeros((NC, NSLC), np.float32)
    jj = np.arange(NSLC)
    for off, wt in ((-1, 1.0), (0, 2.0), (1, 2.0), (2, 2.0), (3, 1.0)):
        n = 4 * jj + off
        ok = (n >= 0) & (n < NC - 1)
        A[n[ok], jj[ok]] = wt
    A = A.astype(BF)
    NCp = (NC + 127) // 128 * 128
    CW = 65 + NSLC

    def buildB(k):
        QT4_d = k.dram("QT4", [2, 128, S], BF16, "ExternalInput")
        Qdup_d = k.dram("Qdup", [2, 128, S], BF16, "ExternalInput")
        KcT2_d = k.dram("KcT2", [128, NCp], BF16, "ExternalInput")
        V1cA_d = k.dram("V1cA", [128, NCp // 128, CW], BF16, "ExternalInput")
        KsKw_d = k.dram("KsKw", [128, S], BF16, "ExternalInput")
        V1s_d = k.dram("V1sh", [128, S // 128, 65], BF16, "ExternalInput")
        V1w_d = k.dram("V1wh", [128, S // 128, 65], BF16, "ExternalInput")
        gate_d = k.dram("gateh", [128, S // 128, 2, 3], F32, "ExternalInput")
        O_d = k.dram("O", [S, 128], BF16, "ExternalOutput")
        oc_d = k.dram("ocs", [S, 128], BF16, "ExternalOutput")
        emit_nsa_attn(k, S, QT4_d, Qdup_d, KcT2_d, V1cA_d, KsKw_d, V1s_d, V1w_d, gate_d, O_d, oc_d, _psum(k))
        return [O_d, oc_d]

    ins = []
    for c in range(NCORES):
        g, sub = c // 2, c % 2
        own = QT[2 * g + sub]
        oth = QT[2 * g + 1 - sub]
        Qdup = np.stack([np.concatenate([own[r * 64:(r + 1) * 64]] * 2, 0) for r in range(2)], 0)
        kc2 = np.zeros((128, NCp), BF)
        kc2[0:64, :NC] = Kc[g]
        kc2[64:128, :NC] = Kc[g]
        vca = np.zeros((NCp, CW), BF)
        vca[:NC, 0:65] = V1c[g]
        vca[:NC, 65:] = A
        gh = gate[:, (2 * c) * 3:(2 * c + 2) * 3].reshape(S // 128, 128, 2, 3).transpose(1, 0, 2, 3)
        ins.append(dict(QT4=np.ascontiguousarray(np.stack([own, oth], 0)), Qdup=np.ascontiguousarray(Qdup), KcT2=kc2,
                        V1cA=_v1_head_layout(vca, NCp), KsKw=np.ascontiguousarray(np.concatenate([KsT[g], KwT[g]], 0)),
                        V1sh=_v1_head_layout(V1s[:, g, :], S), V1wh=_v1_head_layout(V1w[:, g, :], S),
                        gateh=np.ascontiguousarray(gh)))
    rB = _launch(buildB, ins)
    O_all = np.concatenate([r["O"] for r in rB], 1)
    return run_post(S, O_all, h, i, dict(P, wout=P["nsa_w_out"][j]))


def forward(P, S=16384, depth=4):
    P = {n: np.asarray(v) for n, v in P.items()}
    h = np.ascontiguousarray(P["x"][0].astype(np.float32))
    for i in range(depth):
        m, j = i % 3, i // 3
        if m == 0:
            h = run_da(S, h, i, j, P)
        elif m == 1:
            h = run_mla(S, h, i, j, P)
        else:
            h = run_nsa(S, h, i, j, P)
    return h[None].astype(np.float32)


def kernel(**inputs):
    return forward(inputs)
```
